# Optimizing a Trainium2 kernel written in Bass

```python
import jax, jax.numpy as jnp
from jax import lax
import numpy as np

D_MODEL = 2048
BATCH = 8
SEQ = 2048
DEPTH = 2

GRID_W = 64
CTX_LEN = 256
D_FOURIER = D_MODEL // 2
FOURIER_GROUPS = 4
D_LRU = D_MODEL // 2
LRU_HEADS = 8
LRU_HEAD_DIM = D_LRU // LRU_HEADS
CONV_WIDTH = 4
CONV_LEFT = 2
LRU_C = 8.0
N_IN = D_FOURIER + 2 * D_LRU + 2 * D_MODEL
D_FF = 5632
N_EXPERTS = 8
TOP_K = 2
D_EXPERT = 7168
N_DENSE = (DEPTH + 1) // 2
N_MOE = DEPTH // 2
NORM_EPS = 1e-6

kernel_name = "hybrid_fnet_rglru_moe_diffusion_trunk"


def rmsnorm(h, g):
    hf = h.astype(jnp.float32)
    y = hf * lax.rsqrt(jnp.mean(hf * hf, axis=-1, keepdims=True) + NORM_EPS)
    return (y * g.astype(jnp.float32)).astype(h.dtype)


def modulate(h, shift, scale):
    return h * (1.0 + scale) + shift


def ada_mod(cvec, w, b):
    m = jax.nn.silu(cvec) @ w + b
    return jnp.split(m, 6, axis=-1)


def fourier_mix(u):
    B, L, _ = u.shape
    ug = u.astype(jnp.float32).reshape(B, L, FOURIER_GROUPS, D_FOURIER // FOURIER_GROUPS)
    y = jnp.fft.fftn(ug, axes=(1, 3), norm="ortho").real
    return y.reshape(B, L, D_FOURIER).astype(u.dtype)


def dwconv(u, w, b):
    L = u.shape[-2]
    pad = [(0, 0)] * (u.ndim - 2) + [(CONV_LEFT, CONV_WIDTH - 1 - CONV_LEFT), (0, 0)]
    up = jnp.pad(u, pad)
    y = up[..., 0:L, :] * w[0]
    for k in range(1, CONV_WIDTH):
        y = y + up[..., k:k + L, :] * w[k]
    return y + b


def lru_conv_input(ur, conv_w, conv_b, grid):
    if grid:
        B, L, C = ur.shape
        rows = L // GRID_W
        return dwconv(ur.reshape(B, rows, GRID_W, C), conv_w, conv_b).reshape(B, L, C)
    return dwconv(ur, conv_w, conv_b)


def _lin_combine(e1, e2):
    a1, b1 = e1
    a2, b2 = e2
    return a1 * a2, a2 * b1 + b2


def rglru(v, wa, ba, wx, bx, lam, h0, reverse):
    B, L, _ = v.shape
    vf = v.astype(jnp.float32)
    vh = vf.reshape(B, L, LRU_HEADS, LRU_HEAD_DIM)
    r = jax.nn.sigmoid(jnp.einsum('blhi,hij->blhj', vh, wa.astype(jnp.float32)).reshape(B, L, D_LRU) + ba.astype(jnp.float32))
    i = jax.nn.sigmoid(jnp.einsum('blhi,hij->blhj', vh, wx.astype(jnp.float32)).reshape(B, L, D_LRU) + bx.astype(jnp.float32))
    log_a = -LRU_C * r * jax.nn.softplus(-lam.astype(jnp.float32))
    a = jnp.exp(log_a)
    bterm = jnp.sqrt(-jnp.expm1(2.0 * log_a)) * (i * vf)
    A, Bc = lax.associative_scan(_lin_combine, (a, bterm), axis=1, reverse=reverse)
    h = Bc + A * h0[:, None, :]
    h_last = h[:, 0] if reverse else h[:, -1]
    return h, h_last


def mixer(h, w_in, conv_w, conv_b, wa, ba, wx, bx, lam, w_fo, w_ro, w_o, h0f, h0b, grid):
    proj = h @ w_in
    o1 = D_FOURIER
    o2 = o1 + D_LRU
    o3 = o2 + D_LRU
    o4 = o3 + D_MODEL
    uf, ur, ug, gf, gr = proj[..., :o1], proj[..., o1:o2], proj[..., o2:o3], proj[..., o3:o4], proj[..., o4:]
    y_f = fourier_mix(uf) @ w_fo
    v = lru_conv_input(ur, conv_w, conv_b, grid)
    hf, hTf = rglru(v, wa[0], ba[0], wx[0], bx[0], lam[0], h0f, False)
    hb, hTb = rglru(v, wa[1], ba[1], wx[1], bx[1], lam[1], h0b, True)
    y_r = ((hf + hb).astype(h.dtype) * jax.nn.gelu(ug)) @ w_ro
    merged = jax.nn.sigmoid(gf) * y_f + jax.nn.sigmoid(gr) * y_r
    return merged @ w_o, hTf, hTb


def context_lru_states(h, w_in, conv_w, conv_b, wa, ba, wx, bx, lam, h0):
    ur = h @ w_in[:, D_FOURIER:D_FOURIER + D_LRU]
    v = lru_conv_input(ur, conv_w, conv_b, False)
    _, hTf = rglru(v, wa[0], ba[0], wx[0], bx[0], lam[0], h0, False)
    _, hTb = rglru(v, wa[1], ba[1], wx[1], bx[1], lam[1], h0, True)
    return hTf, hTb


def swiglu(h, wg, wu, wd):
    return (jax.nn.silu(h @ wg) * (h @ wu)) @ wd


def moe_swiglu(h, router, wg, wu, wd):
    B, L, D = h.shape
    t = h.reshape(B * L, D)
    logits = (t @ router).astype(jnp.float32)
    top_v, top_i = lax.top_k(logits, TOP_K)
    gates = jax.nn.softmax(top_v, axis=-1)
    combine = jnp.sum(jax.nn.one_hot(top_i, N_EXPERTS, dtype=jnp.float32) * gates[..., None], axis=1)
    out = jnp.zeros_like(t)
    for e in range(N_EXPERTS):
        out = out + combine[:, e:e + 1].astype(t.dtype) * swiglu(t, wg[e], wu[e], wd[e])
    return out.reshape(B, L, D)


def channel_mixer(h, l, ffn_w_gate, ffn_w_up, ffn_w_down, moe_router, moe_w_gate, moe_w_up, moe_w_down):
    if l % 2 == 0:
        j = l // 2
        return swiglu(h, ffn_w_gate[j], ffn_w_up[j], ffn_w_down[j])
    j = l // 2
    return moe_swiglu(h, moe_router[j], moe_w_gate[j], moe_w_up[j], moe_w_down[j])


def setup_inputs(seed: int = 0) -> dict:
    key = jax.random.key(seed)
    ks = jax.random.split(key, 32)
    f32 = jnp.float32
    nrm = lambda k, shape, s: jax.random.normal(k, shape, f32) * s
    u = jax.random.uniform(ks[15], (DEPTH, 2, D_LRU), f32, 0.9, 0.999)
    s = u ** (1.0 / LRU_C)
    lam = jnp.log(s) - jnp.log1p(-s)
    return {
        "x": nrm(ks[0], (BATCH, SEQ, D_MODEL), 1.0),
        "c": nrm(ks[1], (BATCH, D_MODEL), 1.0),
        "ctx": nrm(ks[2], (BATCH, CTX_LEN, D_MODEL), 1.0),
        "c_ctx": nrm(ks[3], (D_MODEL,), 1.0),
        "ada_w": nrm(ks[4], (DEPTH, D_MODEL, 6 * D_MODEL), 0.5 * D_MODEL ** -0.5),
        "ada_b": nrm(ks[5], (DEPTH, 6 * D_MODEL), 0.02),
        "norm1_g": 1.0 + nrm(ks[6], (DEPTH, D_MODEL), 0.02),
        "norm2_g": 1.0 + nrm(ks[7], (DEPTH, D_MODEL), 0.02),
        "w_in": nrm(ks[8], (DEPTH, D_MODEL, N_IN), D_MODEL ** -0.5),
        "conv_w": nrm(ks[9], (DEPTH, CONV_WIDTH, D_LRU), CONV_WIDTH ** -0.5),
        "conv_b": nrm(ks[10], (DEPTH, D_LRU), 0.02),
        "lru_wa": nrm(ks[11], (DEPTH, 2, LRU_HEADS, LRU_HEAD_DIM, LRU_HEAD_DIM), LRU_HEAD_DIM ** -0.5),
        "lru_ba": nrm(ks[12], (DEPTH, 2, D_LRU), 0.02),
        "lru_wx": nrm(ks[13], (DEPTH, 2, LRU_HEADS, LRU_HEAD_DIM, LRU_HEAD_DIM), LRU_HEAD_DIM ** -0.5),
        "lru_bx": nrm(ks[14], (DEPTH, 2, D_LRU), 0.02),
        "lru_lambda": lam,
        "w_fourier_out": nrm(ks[16], (DEPTH, D_FOURIER, D_MODEL), D_FOURIER ** -0.5),
        "w_lru_out": nrm(ks[17], (DEPTH, D_LRU, D_MODEL), D_LRU ** -0.5),
        "w_out": nrm(ks[18], (DEPTH, D_MODEL, D_MODEL), D_MODEL ** -0.5),
        "ffn_w_gate": nrm(ks[19], (N_DENSE, D_MODEL, D_FF), D_MODEL ** -0.5),
        "ffn_w_up": nrm(ks[20], (N_DENSE, D_MODEL, D_FF), D_MODEL ** -0.5),
        "ffn_w_down": nrm(ks[21], (N_DENSE, D_FF, D_MODEL), D_FF ** -0.5),
        "moe_router": nrm(ks[22], (N_MOE, D_MODEL, N_EXPERTS), D_MODEL ** -0.5),
        "moe_w_gate": nrm(ks[23], (N_MOE, N_EXPERTS, D_MODEL, D_EXPERT), D_MODEL ** -0.5),
        "moe_w_up": nrm(ks[24], (N_MOE, N_EXPERTS, D_MODEL, D_EXPERT), D_MODEL ** -0.5),
        "moe_w_down": nrm(ks[25], (N_MOE, N_EXPERTS, D_EXPERT, D_MODEL), D_EXPERT ** -0.5),
        "final_norm_g": 1.0 + nrm(ks[26], (D_MODEL,), 0.02),
    }


def reference(x, c, ctx, c_ctx, ada_w, ada_b, norm1_g, norm2_g, w_in, conv_w, conv_b,
              lru_wa, lru_ba, lru_wx, lru_bx, lru_lambda, w_fourier_out, w_lru_out, w_out,
              ffn_w_gate, ffn_w_up, ffn_w_down, moe_router, moe_w_gate, moe_w_up, moe_w_down,
              final_norm_g):
    lat = x
    cx = ctx
    B = x.shape[0]
    h_zero = jnp.zeros((B, D_LRU), jnp.float32)
    for l in range(DEPTH):
        last = l == DEPTH - 1
        sh1, sc1, g1, sh2, sc2, g2 = [m[:, None, :] for m in ada_mod(c, ada_w[l], ada_b[l])]
        csh1, csc1, cg1, csh2, csc2, cg2 = ada_mod(c_ctx, ada_w[l], ada_b[l])
        lp = (w_in[l], conv_w[l], conv_b[l], lru_wa[l], lru_ba[l], lru_wx[l], lru_bx[l], lru_lambda[l])
        hc = modulate(rmsnorm(cx, norm1_g[l]), csh1, csc1)
        if last:
            hTf, hTb = context_lru_states(hc, *lp, h_zero)
        else:
            oc, hTf, hTb = mixer(hc, *lp, w_fourier_out[l], w_lru_out[l], w_out[l], h_zero, h_zero, False)
            cx = cx + cg1 * oc
        hl = modulate(rmsnorm(lat, norm1_g[l]), sh1, sc1)
        ol, _, _ = mixer(hl, *lp, w_fourier_out[l], w_lru_out[l], w_out[l], hTf, hTb, True)
        lat = lat + g1 * ol
        fp = (ffn_w_gate, ffn_w_up, ffn_w_down, moe_router, moe_w_gate, moe_w_up, moe_w_down)
        if not last:
            cx = cx + cg2 * channel_mixer(modulate(rmsnorm(cx, norm2_g[l]), csh2, csc2), l, *fp)
        lat = lat + g2 * channel_mixer(modulate(rmsnorm(lat, norm2_g[l]), sh2, sc2), l, *fp)
    return rmsnorm(lat, final_norm_g)
```

```python
import numpy as np
import concourse.bass as bass
import concourse.mybir as mybir
from concourse.bass_utils import run_bass_kernel_spmd

F32 = mybir.dt.float32
BF16 = mybir.dt.bfloat16
U8 = mybir.dt.uint8
AF = mybir.ActivationFunctionType
ALU = mybir.AluOpType
AX = mybir.AxisListType

D = 2048
KC = 16
SEQ = 2048
CTX = 256
NTOK = CTX + SEQ
DEPTH = 2
N_IN = 7168
D_FF = 5632
D_EXP = 7168
NEXP = 8
EPS = 1e-6
TT = 512

P_ADAB = 0
P_N1 = 96
P_N2 = 112
P_CW = 128
P_CB = 160
P_BA = 168
P_BX = 184
P_LAM = 200
P_LSZ = 216
P_FIN = DEPTH * P_LSZ
NPAR = P_FIN + 16

DEBUG = {"stop_after": None, "dump": False}


class Buf:
    __slots__ = ("name", "writers", "readers", "prev_readers", "dsem", "dcount")

    def __init__(self, name):
        self.name = name
        self.writers = []
        self.readers = []
        self.prev_readers = []
        self.dsem = None
        self.dcount = 0


class Op:
    __slots__ = ("eng", "fn", "raw", "war", "dma", "dst", "dval", "sig", "idx")

    def __init__(self, eng, fn):
        self.eng = eng
        self.fn = fn
        self.raw = []
        self.war = []
        self.dma = False
        self.dst = None
        self.dval = 0
        self.sig = False
        self.idx = 0


class Prog:
    ENGS = ("pe", "act", "dve", "pool", "sp")

    def __init__(self, nc):
        self.nc = nc
        self.q = {e: [] for e in self.ENGS}
        self.bar = Buf("BAR")
        self.nops = 0

    @staticmethod
    def _add_reader(lst, o):
        if not o.dma:
            for i, r in enumerate(lst):
                if (not r.dma) and r.eng == o.eng:
                    lst[i] = o
                    return
        lst.append(o)

    def op(self, eng, fn, reads=(), writes=(), pwrites=(), dma_dst=None, barrier=False):
        o = Op(eng, fn)
        if dma_dst is not None:
            o.dma = True
            o.dst = dma_dst
            dma_dst.dcount += 1
            o.dval = 16 * dma_dst.dcount
        rd = list(reads)
        wr = list(writes)
        if barrier:
            wr.append(self.bar)
        else:
            rd.append(self.bar)
        for b in rd:
            o.raw.extend(b.writers)
            self._add_reader(b.readers, o)
        for b in wr:
            o.raw.extend(b.writers)
            if b is self.bar:
                o.raw.extend(b.readers)
            else:
                o.war.extend(b.readers)
            o.war.extend(b.prev_readers)
            b.writers = [o]
            b.readers = []
            b.prev_readers = []
        for b in pwrites:
            o.war.extend(b.readers)
            o.war.extend(b.prev_readers)
            if b.readers:
                b.prev_readers = b.readers
                b.readers = []
                b.writers = []
            self._add_reader(b.writers, o)
        self.q[eng].append(o)
        self.nops += 1
        return o

    def barrier(self):
        self.op("dve", None, barrier=True)

    def emit(self, block_engines, sems):
        for e in self.ENGS:
            for o in self.q[e]:
                for d in o.raw + o.war:
                    if not d.dma:
                        d.sig = True
        for e in self.ENGS:
            n = 0
            for o in self.q[e]:
                if o.sig and not o.dma:
                    n += 1
                    o.idx = n
        for e in self.ENGS:
            eng = block_engines[e]
            waited = {}
            pending_inc = [0]

            def need(sem, val, waited=waited, eng=eng):
                if waited.get(sem.name, 0) < val:
                    eng.wait_ge(sem, val)
                    waited[sem.name] = val

            for o in self.q[e]:
                for d, is_war in [(x, False) for x in o.raw] + [(x, True) for x in o.war]:
                    if d.dma:
                        need(d.dst.dsem, d.dval)
                    else:
                        if d.eng == e:
                            if e in ("pe", "sp", "pool") or is_war:
                                continue
                        need(sems[d.eng], d.idx)
                ins = o.fn(eng) if o.fn is not None else None
                if o.dma:
                    ins.then_inc(o.dst.dsem, 16)
                elif o.sig:
                    if ins is None:
                        ins = eng.nop()
                    ins.then_inc(sems[e], 1)


def build_program(stop_after=None, dump=False):
    nc = bass.Bass("TRN2", target_bir_lowering=False)
    from contextlib import ExitStack
    es = ExitStack()

    def din(name, shape, dt=F32):
        return nc.dram_tensor(name, list(shape), dt, kind="ExternalInput").ap()

    def dscr(name, shape, dt=F32):
        return nc.dram_tensor(name, list(shape), dt, kind=("ExternalOutput" if dump else "Internal")).ap()

    x_d = din("x", [SEQ, D])
    ctx_d = din("ctx", [CTX, D])
    cc_d = din("cc", [128, KC, 2])
    par_d = din("params", [128, NPAR])
    ident_d = din("ident", [128, 128])
    cs_d = din("cs256", [256, 512])
    tpl_d = din("tp_lat", [2, SEQ, SEQ])
    tpc_d = din("tp_ctx", [2, CTX, CTX])
    ada_w = din("ada_w", [DEPTH, D, 6 * D])
    w_in = din("w_in", [DEPTH, D, N_IN])
    lru_wa = din("lru_wa", [DEPTH, 2, 8, 128, 128])
    lru_wx = din("lru_wx", [DEPTH, 2, 8, 128, 128])
    w_fo = din("w_fourier_out", [DEPTH, 1024, D])
    w_ro = din("w_lru_out", [DEPTH, 1024, D])
    w_o = din("w_out", [DEPTH, D, D])
    ffn_g = din("ffn_w_gate", [1, D, D_FF])
    ffn_u = din("ffn_w_up", [1, D, D_FF])
    ffn_d = din("ffn_w_down", [1, D_FF, D])
    router_d = din("moe_router", [1, D, NEXP])
    moe_g = din("moe_w_gate", [1, NEXP, D, D_EXP])
    moe_u = din("moe_w_up", [1, NEXP, D, D_EXP])
    moe_d = din("moe_w_down", [1, NEXP, D_EXP, D])
    out_d = nc.dram_tensor("out", [SEQ, D], F32, kind="ExternalOutput").ap()

    resT = {"lat": dscr("resT_lat", [KC, 128, SEQ]), "ctx": dscr("resT_ctx", [KC, 128, CTX])}
    urT = dscr("urT", [8, 128, NTOK])
    ggT = dscr("ggT", [8, 128, NTOK])
    sgfT = dscr("sgfT", [KC, 128, NTOK])
    sgrT = dscr("sgrT", [KC, 128, NTOK])
    Pd = {"lat": dscr("P_lat", [SEQ, 2, 1024], BF16), "ctx": dscr("P_ctx", [CTX, 2, 1024], BF16)}
    YTd = dscr("YT", [8, 128, NTOK], BF16)
    zTd = dscr("zT", [8, 128, NTOK], BF16)
    mod_dbg = dscr("mod_dbg", [128, DEPTH * 2 * 96]) if dump else None

    P = Prog(nc)

    def sb(name, shape, dt):
        return es.enter_context(nc.sbuf_tensor("s_" + name, list(shape), dt))

    WSLOTS = 4
    wslot = [sb("wslot%d" % i, [128, 16, 512], BF16) for i in range(WSLOTS)]
    wslot_b = [Buf("wslot%d" % i) for i in range(WSLOTS)]
    wctr = [0]

    def next_slot():
        i = wctr[0] % WSLOTS
        wctr[0] += 1
        return wslot[i], wslot_b[i]

    res = sb("res", [128, KC, TT], F32)
    res_b = [Buf("res%d" % k) for k in range(KC)]
    ident = sb("ident", [128, 128], F32)
    ones = sb("ones", [128, 128], F32)
    cs_sb = sb("cs_sb", [128, 2, 512], BF16)
    par = sb("par", [128, NPAR], F32)
    mod = sb("mod", [128, DEPTH * 2 * 96], F32)
    drv = sb("drv", [128, DEPTH * 2 * 64], F32)
    s8 = sb("s8", [128, DEPTH * 32], F32)
    cc_sb = sb("cc_sb", [128, KC, 2], F32)
    sc_bf = sb("sc_bf", [128, KC, 2], BF16)
    router_sb = sb("router_sb", [128, KC, NEXP], F32)
    epsb = sb("epsb", [128, 1], F32)
    oneb = sb("oneb", [128, 1], F32)
    ARENA = 100 * 1024
    arena = sb("arena", [128, ARENA], U8)
    consts_b = Buf("consts")
    par_b = Buf("par")
    mod_b = Buf("mod")
    drv_b = Buf("drv")

    class Carve:
        def __init__(self):
            self.off = 0

        def reset(self):
            self.off = 0

        def take(self, nelem, dt, shape=None):
            esz = 4 if dt == F32 else 2
            nb = nelem * esz
            nb = (nb + 63) // 64 * 64
            assert self.off + nb <= ARENA, ("arena overflow", self.off, nb)
            a = arena[:, self.off:self.off + nelem * esz].bitcast(dt)
            self.off += nb
            return a

    cv = Carve()
    _pb = {}

    def pb(name):
        if name not in _pb:
            _pb[name] = Buf(name)
        return _pb[name]

    psum = [es.enter_context(nc.psum_tensor("ps%d" % i, [128, 512], F32)) for i in range(8)]
    psum_b = [Buf("ps%d" % i) for i in range(8)]
    pctr = [0]

    def next_bank():
        i = pctr[0] % 8
        pctr[0] += 1
        return psum[i], psum_b[i]

    ectr = [0]

    def ev_eng():
        ectr[0] += 1
        return "act" if ectr[0] % 2 else "dve"

    def dma(eng, out_ap, in_ap, dst_buf, reads=(), writes=(), pwrites=()):
        if eng == "pool":
            fn = lambda e, o=out_ap, i=in_ap: e.dma_start(out=o, in_=i)
        else:
            fn = lambda e, o=out_ap, i=in_ap: e.dma_start(out=o, in_=i)
        return P.op(eng, fn, reads=reads, writes=writes, pwrites=pwrites, dma_dst=dst_buf)

    def load_w(dram_ap_2d, k0, nk, c0, ncol, slot, slot_buf, kc_off=0, first=True):
        src = dram_ap_2d[k0 * 128:(k0 + nk) * 128, c0:c0 + ncol].rearrange("(k p) n -> p k n", p=128)
        dst = slot[:, kc_off:kc_off + nk, 0:ncol]
        if first:
            return dma("pool", dst, src, slot_buf, writes=[slot_buf])
        return dma("pool", dst, src, slot_buf, pwrites=[slot_buf])

    def mm_group(bank, bank_b, cols, pairs, reads, first=True, last=True, fresh=None):
        if fresh is None:
            fresh = first
        def fn(e, bank=bank, cols=cols, pairs=pairs, first=first, last=last):
            n = len(pairs)
            ins = None
            for i, (l, r) in enumerate(pairs):
                ins = e.matmul(bank[:, cols[0]:cols[1]], l, r, start=(first and i == 0), stop=(last and i == n - 1))
            return ins
        if fresh:
            return P.op("pe", fn, reads=reads, writes=[bank_b])
        return P.op("pe", fn, reads=reads, pwrites=[bank_b])

    def act_op(out_ap, in_ap, func, reads, writes, bias=None, scale=None, eng="act"):
        kw = {}
        if bias is not None:
            kw["bias"] = bias
        if scale is not None:
            kw["scale"] = scale
        return P.op("act", lambda e, o=out_ap, i=in_ap, f=func, kw=kw: e.activation(out=o, in_=i, func=f, **kw),
                    reads=reads, writes=writes)

    def copy_op(eng, out_ap, in_ap, reads, writes):
        if eng == "act":
            return P.op("act", lambda e, o=out_ap, i=in_ap: e.activation(out=o, in_=i, func=AF.Copy), reads=reads, writes=writes)
        return P.op("dve", lambda e, o=out_ap, i=in_ap: e.tensor_copy(out=o, in_=i), reads=reads, writes=writes)

    def dve(fn, reads, writes):
        return P.op("dve", fn, reads=reads, writes=writes)

    dma("sp", ident[:], ident_d, consts_b, writes=[consts_b])
    dma("sp", par[:], par_d, par_b, writes=[par_b])
    cc_b = Buf("cc")
    dma("sp", cc_sb[:], cc_d, cc_b, writes=[cc_b])
    rt_b = Buf("router")
    dma("sp", router_sb[:], router_d[0].rearrange("(k p) e -> p k e", p=128), rt_b, writes=[rt_b])
    csb_b = Buf("cs")
    dma("pool", cs_sb[:], cs_d.rearrange("(c p) n -> p c n", p=128), csb_b, writes=[csb_b])
    misc_b = Buf("misc")
    dve(lambda e: e.memset(ones[:], 1.0), [], [misc_b])
    dve(lambda e: e.memset(epsb[:], EPS), [], [misc_b])
    dve(lambda e: e.memset(oneb[:], 1.0), [], [misc_b])

    def tile_list(S):
        if S == "ctx":
            return [(0, CTX)]
        return [(t * TT, TT) for t in range(SEQ // TT)]

    COL0 = {"ctx": 0, "lat": CTX}
    SIDX = {"lat": 0, "ctx": 1}
    resT_b = {S: [Buf("resT_%s%d" % (S, i)) for i in range(len(tile_list(S)))] for S in ("ctx", "lat")}
    proj_b = {S: [Buf("proj_%s%d" % (S, i)) for i in range(len(tile_list(S)))] for S in ("ctx", "lat")}
    Pd_b = {S: Buf("Pd_%s" % S) for S in ("ctx", "lat")}
    YT_b = {S: [Buf("YT_%s%d" % (S, i)) for i in range(len(tile_list(S)))] for S in ("ctx", "lat")}
    zT_b = Buf("zT")
    out_b = Buf("out")

    def phase0():
        cv.reset()
        xin = cv.take(4 * D, F32).rearrange("p (a f) -> p a f", a=4)
        xin_b = pb("xin")
        for S, src in (("ctx", ctx_d), ("lat", x_d)):
            for ti, (t0, T) in enumerate(tile_list(S)):
                ntb = T // 128
                dma("sp", xin[:, 0:ntb, :], src[t0:t0 + T, :].rearrange("(a p) f -> p a f", p=128), xin_b, writes=[xin_b])
                for k in range(KC):
                    bank, bb = next_bank()

                    def fn(e, bank=bank, k=k, ntb=ntb):
                        ins = None
                        for tb in range(ntb):
                            ins = e.transpose(bank[:, tb * 128:(tb + 1) * 128], xin[:, tb, k * 128:(k + 1) * 128], ident[:])
                        return ins
                    P.op("pe", fn, reads=[xin_b, consts_b], writes=[bb])
                    copy_op(ev_eng(), res[:, k, 0:T], bank[:, 0:T], [bb], [res_b[k]])
                dma("sp", resT[S].rearrange("k p t -> p k t")[:, :, t0:t0 + T], res[:, :, 0:T], resT_b[S][ti],
                    reads=res_b, writes=[resT_b[S][ti]])

    def phase_ada(l):
        sc_b = Buf("sc_bf")
        act_op(sc_bf[:], cc_sb[:], AF.Silu, [cc_b], [sc_b])
        adab = par[:, l * P_LSZ + P_ADAB:l * P_LSZ + P_ADAB + 96]
        modl = mod[:, l * 192:(l + 1) * 192].rearrange("p (s n) -> p s n", s=2)
        for pn in range(24):
            slot, slb = next_slot()
            load_w(ada_w[l], 0, KC, pn * 512, 512, slot, slb)
            bank, bb = next_bank()
            for j in range(4):
                pairs = [(slot[:, k, j * 128:(j + 1) * 128], sc_bf[:, k, :]) for k in range(KC)]
                mm_group(bank, bb, (2 * j, 2 * j + 2), pairs, [slb, sc_b], fresh=(j == 0))
            for j in range(4):
                n = pn * 4 + j
                P.op("dve", lambda e, o=modl[:, :, n], i=bank[:, 2 * j:2 * j + 2], s=adab[:, n:n + 1]:
                     e.tensor_scalar(out=o, in0=i, scalar1=s, scalar2=None, op0=ALU.add),
                     reads=[bb, par_b], pwrites=[mod_b])
        for s in range(2):
            for which, (goff, scoff) in enumerate(((P_N1, 16), (P_N2, 64))):
                o = drv[:, (l * 2 + s) * 64 + which * 16:(l * 2 + s) * 64 + which * 16 + 16]
                scv = modl[:, s, scoff:scoff + 16]
                g = par[:, l * P_LSZ + goff:l * P_LSZ + goff + 16]
                P.op("dve", lambda e, o=o, scv=scv, g=g: e.scalar_tensor_tensor(out=o, in0=scv, scalar=1.0, in1=g, op0=ALU.add, op1=ALU.mult),
                     reads=[mod_b, par_b], pwrites=[drv_b])
        lam = par[:, l * P_LSZ + P_LAM:l * P_LSZ + P_LAM + 16]
        t = drv[:, (l * 2) * 64 + 32:(l * 2) * 64 + 48]
        u = drv[:, (l * 2) * 64 + 48:(l * 2) * 64 + 64]
        t2 = drv[:, (l * 2 + 1) * 64 + 32:(l * 2 + 1) * 64 + 48]
        t3 = drv[:, (l * 2 + 1) * 64 + 48:(l * 2 + 1) * 64 + 64]
        tb_ = Buf("s8tmp")
        s8_b = s8_bufs[l]
        act_op(t, lam, AF.Exp, [par_b], [tb_], scale=-1.0)
        dve(lambda e: e.tensor_scalar(out=u, in0=t, scalar1=1.0, scalar2=None, op0=ALU.add), [tb_], [tb_])
        dve(lambda e: e.tensor_scalar(out=t2, in0=u, scalar1=-1.0, scalar2=1e-30, op0=ALU.add, op1=ALU.max), [tb_], [tb_])
        dve(lambda e: e.reciprocal(out=t2, in_=t2), [tb_], [tb_])
        act_op(t3, u, AF.Ln, [tb_], [tb_])
        dve(lambda e: e.tensor_tensor(out=t3, in0=t3, in1=t, op=ALU.mult), [tb_], [tb_])
        dve(lambda e: e.scalar_tensor_tensor(out=s8[:, l * 32:l * 32 + 16], in0=t3, scalar=-8.0, in1=t2, op0=ALU.mult, op1=ALU.mult), [tb_], [s8_b])
        dve(lambda e: e.tensor_scalar(out=s8[:, l * 32 + 16:l * 32 + 32], in0=s8[:, l * 32:l * 32 + 16], scalar1=2.0, scalar2=None, op0=ALU.mult), [s8_b], [s8_b])

    s8_bufs = [Buf("s8_%d" % l) for l in range(DEPTH)]

    def modv(l, S, idx):
        base = l * 192 + SIDX[S] * 96 + idx * 16
        return mod[:, base:base + 16]

    def drvA(l, S, which):
        base = (l * 2 + SIDX[S]) * 64 + which * 16
        return drv[:, base:base + 16]

    def norm_mod(T, Avec, Bvec, hT, hT_b, scratch, extra=None):
        sq, sq_b, xs, xs_b, rstd, rstd_b = scratch
        bank, bb = next_bank()
        for k in range(KC):
            i2 = k % 2
            act_op(sq[i2][:, 0:T], res[:, k, 0:T], AF.Square, [res_b[k]], [sq_b[i2]])
            mm_group(bank, bb, (0, T), [(ones[:], sq[i2][:, 0:T])], [sq_b[i2], misc_b], first=(k == 0), last=(k == KC - 1))
        act_op(rstd[:, 0:T], bank[:, 0:T], AF.Sqrt, [bb, misc_b], [rstd_b], bias=epsb[:], scale=1.0 / D)
        dve(lambda e: e.reciprocal(out=rstd[:, 0:T], in_=rstd[:, 0:T]), [rstd_b], [rstd_b])
        for k in range(KC):
            i2 = k % 2
            dve(lambda e, k=k, i2=i2: e.scalar_tensor_tensor(out=xs[i2][:, 0:T], in0=res[:, k, 0:T], scalar=Avec[:, k:k + 1],
                                                               in1=rstd[:, 0:T], op0=ALU.mult, op1=ALU.mult),
                [res_b[k], rstd_b, drv_b, par_b], [xs_b[i2]])
            if Bvec is not None:
                act_op(hT[:, k, 0:T], xs[i2][:, 0:T], AF.Identity, [xs_b[i2], mod_b], [hT_b[k]], bias=Bvec[:, k:k + 1])
            if extra is not None:
                extra(k, xs[i2], xs_b[i2])

    def norm_scratch():
        sq = [cv.take(TT, F32) for _ in range(2)]
        xs = [cv.take(TT, F32) for _ in range(2)]
        rstd = cv.take(TT, F32)
        return (sq, [Buf("sq0"), Buf("sq1")], xs, [Buf("xs0"), Buf("xs1")], rstd, Buf("rstd"))

    def phaseA(l, S, ti, t0, T, only_ur):
        cv.reset()
        hT = cv.take(KC * TT, BF16).rearrange("p (k t) -> p k t", k=KC)
        hT_b = [Buf("hT%d" % k) for k in range(KC)]
        scr = norm_scratch()
        ufT = cv.take(8 * TT, BF16).rearrange("p (k t) -> p k t", k=8)
        ufT_b = [Buf("ufT%d" % k) for k in range(8)]
        Pt = cv.take(4 * 2048, BF16).rearrange("p (a c j) -> p a c j", a=4, c=2)
        Pt_b = Buf("Pt")
        stage = [cv.take(4 * TT, F32).rearrange("p (j t) -> p j t", j=4) for _ in range(2)]
        stage_b = [[Buf("st%d_%d" % (i, j)) for j in range(4)] for i in range(2)]
        dma("sp", res[:, :, 0:T], resT[S].rearrange("k p t -> p k t")[:, :, t0:t0 + T], res_b[0],
            reads=[resT_b[S][ti]], writes=res_b)
        norm_mod(T, drvA(l, S, 0), modv(l, S, 0), hT, hT_b, scr)
        c0 = COL0[S] + t0
        panels = [2, 3] if only_ur else list(range(14))
        sctr = 0
        for pn in panels:
            slot, slb = next_slot()
            load_w(w_in[l], 0, KC, pn * 512, 512, slot, slb)
            seg = pn // 2 if pn < 6 else (3 if pn < 10 else 4)
            sti = sctr % 2
            for j in range(4):
                n = pn * 4 + j
                bank, bb = next_bank()
                pairs = [(slot[:, k, j * 128:(j + 1) * 128], hT[:, k, 0:T]) for k in range(KC)]
                mm_group(bank, bb, (0, T), pairs, [slb] + hT_b)
                if seg == 0:
                    copy_op(ev_eng(), ufT[:, n, 0:T], bank[:, 0:T], [bb], [ufT_b[n]])
                elif seg == 1:
                    copy_op("dve", stage[sti][:, j, 0:T], bank[:, 0:T], [bb], [stage_b[sti][j]])
                elif seg == 2:
                    act_op(stage[sti][:, j, 0:T], bank[:, 0:T], AF.Gelu_apprx_tanh, [bb], [stage_b[sti][j]])
                else:
                    act_op(stage[sti][:, j, 0:T], bank[:, 0:T], AF.Sigmoid, [bb], [stage_b[sti][j]])
            if seg == 0:
                if pn == 1:
                    for tb in range(T // 128):
                        for g in range(4):
                            bank, bb = next_bank()
                            pairs = [(ufT[:, 2 * g + c2, tb * 128:(tb + 1) * 128], cs_sb[:, c2, :]) for c2 in range(2)]
                            mm_group(bank, bb, (0, 512), pairs, [ufT_b[2 * g], ufT_b[2 * g + 1], csb_b])
                            o = Pt[:, tb, :, g * 256:(g + 1) * 256]
                            i = bank[:, 0:512].rearrange("p (c j) -> p c j", c=2)
                            eng = ev_eng()
                            if eng == "act":
                                P.op("act", lambda e, o=o, i=i: e.activation(out=o, in_=i, func=AF.Copy), reads=[bb], pwrites=[Pt_b])
                            else:
                                P.op("dve", lambda e, o=o, i=i: e.tensor_copy(out=o, in_=i), reads=[bb], pwrites=[Pt_b])
                    dma("sp", Pd[S][t0:t0 + T].rearrange("(a p) c j -> p a c j", p=128), Pt[:, 0:T // 128], Pd_b[S],
                        reads=[Pt_b], pwrites=[Pd_b[S]])
            else:
                if seg == 1:
                    dst = urT[(pn - 2) * 4:(pn - 2) * 4 + 4]
                elif seg == 2:
                    dst = ggT[(pn - 4) * 4:(pn - 4) * 4 + 4]
                elif seg == 3:
                    dst = sgfT[(pn - 6) * 4:(pn - 6) * 4 + 4]
                else:
                    dst = sgrT[(pn - 10) * 4:(pn - 10) * 4 + 4]
                dma("sp", dst.rearrange("k p t -> p k t")[:, :, c0:c0 + T], stage[sti][:, :, 0:T], proj_b[S][ti],
                    reads=stage_b[sti], pwrites=[proj_b[S][ti]])
                sctr += 1

    def phaseB(l):
        cv.reset()
        N = NTOK
        ur = cv.take(N, F32)
        gg = cv.take(N, F32)
        v = cv.take(N, F32)
        vbf = cv.take(N, BF16)
        r_ = cv.take(N, F32)
        i_ = cv.take(N, F32)
        a_ = cv.take(N, F32)
        m_ = cv.take(N, F32)
        hf = cv.take(N, F32)
        zst = cv.take(N, BF16)
        gw = cv.take(4 * 128, BF16).rearrange("p (g j) -> p g j", g=4)
        v_b, vbf_b, r_b, i_b, a_b, m_b, hf_b, zst_b = [Buf(n) for n in ("v", "vbf", "r", "i", "a", "m", "hf", "zst")]
        ur_b, gg_b, gw_b = pb("ur"), pb("gg"), pb("gw")
        allproj = proj_b["ctx"] + proj_b["lat"]
        lb = l * P_LSZ
        blocks = [(0, CTX)] + [(CTX + t * TT, TT) for t in range(SEQ // TT)]
        for c in range(8):
            dma("sp", ur[:], urT[c], ur_b, reads=allproj, writes=[ur_b])
            dma("sp", gg[:], ggT[c], gg_b, reads=allproj, writes=[gg_b])
            dma("pool", gw[:, 0:2, :], lru_wa[l, :, c].rearrange("d i j -> i d j"), gw_b, writes=[gw_b])
            dma("pool", gw[:, 2:4, :], lru_wx[l, :, c].rearrange("d i j -> i d j"), gw_b, pwrites=[gw_b])
            cwl = [par[:, lb + P_CW + k * 8 + c:lb + P_CW + k * 8 + c + 1] for k in range(4)]
            cw = lambda k, cwl=cwl: cwl[k]
            cb = par[:, lb + P_CB + c:lb + P_CB + c + 1]
            dve(lambda e, cw=cw, cb=cb: e.tensor_scalar(out=v[:], in0=ur[:], scalar1=cw(2), scalar2=cb, op0=ALU.mult, op1=ALU.add),
                [ur_b, par_b], [v_b])
            for k in (0, 1, 3):
                off = k - 2
                lo = max(0, -off)
                hi = max(0, off)
                dve(lambda e, cw=cw, k=k, lo=lo, hi=hi, off=off: e.scalar_tensor_tensor(
                    out=v[:, lo:CTX - hi], in0=ur[:, lo + off:CTX - hi + off], scalar=cw(k), in1=v[:, lo:CTX - hi],
                    op0=ALU.mult, op1=ALU.add), [ur_b, par_b, v_b], [v_b])
                v3 = v[:, CTX:N].rearrange("p (r w) -> p r w", w=64)
                u3 = ur[:, CTX:N].rearrange("p (r w) -> p r w", w=64)
                dve(lambda e, cw=cw, k=k, lo=lo, hi=hi, off=off, v3=v3, u3=u3: e.scalar_tensor_tensor(
                    out=v3[:, :, lo:64 - hi], in0=u3[:, :, lo + off:64 - hi + off], scalar=cw(k), in1=v3[:, :, lo:64 - hi],
                    op0=ALU.mult, op1=ALU.add), [ur_b, par_b, v_b], [v_b])
            copy_op("act", vbf[:], v[:], [v_b], [vbf_b])
            hb = None
            for d in range(2):
                for gi, (dst, dst_b, boff) in enumerate(((r_, r_b, P_BA), (i_, i_b, P_BX))):
                    bias = par[:, lb + boff + d * 8 + c:lb + boff + d * 8 + c + 1]
                    for bi, (b0, bw) in enumerate(blocks):
                        bank, bb = next_bank()
                        mm_group(bank, bb, (0, bw), [(gw[:, gi * 2 + d, :], vbf[:, b0:b0 + bw])], [gw_b, vbf_b])
                        P.op("act", lambda e, o=dst[:, b0:b0 + bw], i=bank[:, 0:bw], bias=bias: e.activation(out=o, in_=i, func=AF.Sigmoid, bias=bias),
                             reads=[bb, par_b], pwrites=[dst_b])
                sc1 = s8[:, l * 32 + d * 8 + c:l * 32 + d * 8 + c + 1]
                sc2 = s8[:, l * 32 + 16 + d * 8 + c:l * 32 + 16 + d * 8 + c + 1]
                act_op(a_[:], r_[:], AF.Exp, [r_b, s8_bufs[l]], [a_b], scale=sc1)
                act_op(m_[:], r_[:], AF.Exp, [r_b, s8_bufs[l]], [m_b], scale=sc2)
                act_op(m_[:], m_[:], AF.Sqrt, [m_b, misc_b], [m_b], bias=oneb[:], scale=-1.0)
                dve(lambda e: e.tensor_tensor(out=i_[:], in0=i_[:], in1=v[:], op=ALU.mult), [i_b, v_b], [i_b])
                dve(lambda e: e.tensor_tensor(out=i_[:], in0=i_[:], in1=m_[:], op=ALU.mult), [i_b, m_b], [i_b])
                if d == 0:
                    dve(lambda e: e.tensor_tensor_scan(out=hf[:], data0=a_[:], data1=i_[:], initial=0.0, op0=ALU.mult, op1=ALU.add),
                        [a_b, i_b], [hf_b])
                else:
                    dve(lambda e: e.tensor_tensor_scan(out=r_[:, 0:CTX][:, ::-1], data0=a_[:, 0:CTX][:, ::-1], data1=i_[:, 0:CTX][:, ::-1],
                                                       initial=0.0, op0=ALU.mult, op1=ALU.add), [a_b, i_b, r_b], [r_b])
                    dve(lambda e: e.tensor_tensor_scan(out=r_[:, CTX:N][:, ::-1], data0=a_[:, CTX:N][:, ::-1], data1=i_[:, CTX:N][:, ::-1],
                                                       initial=r_[:, 0:1], op0=ALU.mult, op1=ALU.add), [a_b, i_b, r_b], [r_b])
            dve(lambda e: e.tensor_tensor(out=hf[:], in0=hf[:], in1=r_[:], op=ALU.add), [hf_b, r_b], [hf_b])
            dve(lambda e: e.tensor_tensor(out=zst[:], in0=hf[:], in1=gg[:], op=ALU.mult), [hf_b, gg_b], [zst_b])
            dma("sp", zTd[c], zst[:], zT_b, reads=[zst_b], pwrites=[zT_b])

    def phaseF(l, S):
        cv.reset()
        L = CTX if S == "ctx" else SEQ
        ntc = L // 128
        Psb = cv.take(ntc * 2048, BF16).rearrange("p (a c j) -> p a c j", a=ntc, c=2)
        Psb_b = pb("Psb")
        yst = [cv.take(8 * TT, BF16).rearrange("p (k t) -> p k t", k=8) for _ in range(2)]
        yst_b = [[Buf("yst%d_%d" % (i, k)) for k in range(8)] for i in range(2)]
        tp = tpc_d if S == "ctx" else tpl_d
        for a0 in range(0, ntc, 4):
            na = min(4, ntc - a0)
            dma("sp", Psb[:, a0:a0 + na], Pd[S][a0 * 128:(a0 + na) * 128].rearrange("(a p) c j -> p a c j", p=128), Psb_b,
                reads=[Pd_b[S]], pwrites=[Psb_b])
        for ti, (t0, T) in enumerate(tile_list(S)):
            slots = []
            for cs in range(2):
                slot, slb = next_slot()
                src = tp[cs, :, t0:t0 + T].rearrange("(a p) k -> p a k", p=128)
                dma("pool", slot[:, 0:ntc, 0:T], src, slb, writes=[slb])
                slots.append((slot, slb))
            si = ti % 2
            for jc in range(8):
                bank, bb = next_bank()
                pairs = []
                for cs in range(2):
                    for a in range(ntc):
                        pairs.append((Psb[:, a, cs, jc * 128:(jc + 1) * 128], slots[cs][0][:, a, 0:T]))
                mm_group(bank, bb, (0, T), pairs, [Psb_b, slots[0][1], slots[1][1]])
                copy_op(ev_eng(), yst[si][:, jc, 0:T], bank[:, 0:T], [bb], [yst_b[si][jc]])
            c0 = COL0[S] + t0
            dma("sp", YTd.rearrange("k p t -> p k t")[:, :, c0:c0 + T], yst[si][:, :, 0:T], YT_b[S][ti],
                reads=yst_b[si], writes=[YT_b[S][ti]])

    def phaseC(l, S, ti, t0, T):
        cv.reset()
        c0 = COL0[S] + t0
        yt = cv.take(8 * TT, BF16).rearrange("p (k t) -> p k t", k=8)
        zt = cv.take(8 * TT, BF16).rearrange("p (k t) -> p k t", k=8)
        sgf = [cv.take(4 * TT, F32).rearrange("p (j t) -> p j t", j=4) for _ in range(2)]
        sgr = [cv.take(4 * TT, F32).rearrange("p (j t) -> p j t", j=4) for _ in range(2)]
        t1 = [cv.take(TT, F32) for _ in range(2)]
        t2 = [cv.take(TT, F32) for _ in range(2)]
        mg = cv.take(KC * TT, BF16).rearrange("p (k t) -> p k t", k=KC)
        yt_b, zt_b = pb("yt"), pb("zt")
        sgf_b = [pb("sgf0"), pb("sgf1")]
        sgr_b = [pb("sgr0"), pb("sgr1")]
        t1_b = [Buf("t1_0"), Buf("t1_1")]
        t2_b = [Buf("t2_0"), Buf("t2_1")]
        mg_b = [Buf("mg%d" % k) for k in range(KC)]
        dma("sp", res[:, :, 0:T], resT[S].rearrange("k p t -> p k t")[:, :, t0:t0 + T], res_b[0],
            reads=[resT_b[S][ti]], writes=res_b)
        dma("sp", yt[:, :, 0:T], YTd.rearrange("k p t -> p k t")[:, :, c0:c0 + T], yt_b, reads=[YT_b[S][ti]], writes=[yt_b])
        dma("sp", zt[:, :, 0:T], zTd.rearrange("k p t -> p k t")[:, :, c0:c0 + T], zt_b, reads=[zT_b], writes=[zt_b])
        for pn in range(4):
            slot, slb = next_slot()
            load_w(w_fo[l], 0, 8, pn * 512, 512, slot, slb, kc_off=0, first=True)
            load_w(w_ro[l], 0, 8, pn * 512, 512, slot, slb, kc_off=8, first=False)
            si = pn % 2
            dma("sp", sgf[si][:, :, 0:T], sgfT[pn * 4:pn * 4 + 4].rearrange("k p t -> p k t")[:, :, c0:c0 + T], sgf_b[si],
                reads=[proj_b[S][ti]], writes=[sgf_b[si]])
            dma("sp", sgr[si][:, :, 0:T], sgrT[pn * 4:pn * 4 + 4].rearrange("k p t -> p k t")[:, :, c0:c0 + T], sgr_b[si],
                reads=[proj_b[S][ti]], writes=[sgr_b[si]])
            for j in range(4):
                n = pn * 4 + j
                i2 = n % 2
                bF, bFb = next_bank()
                bR, bRb = next_bank()
                mm_group(bF, bFb, (0, T), [(slot[:, k, j * 128:(j + 1) * 128], yt[:, k, 0:T]) for k in range(8)], [slb, yt_b])
                mm_group(bR, bRb, (0, T), [(slot[:, 8 + k, j * 128:(j + 1) * 128], zt[:, k, 0:T]) for k in range(8)], [slb, zt_b])
                dve(lambda e, o=t1[i2], b=bF, s=sgf[si], j=j: e.tensor_tensor(out=o[:, 0:T], in0=b[:, 0:T], in1=s[:, j, 0:T], op=ALU.mult),
                    [bFb, sgf_b[si]], [t1_b[i2]])
                dve(lambda e, o=t2[i2], b=bR, s=sgr[si], j=j: e.tensor_tensor(out=o[:, 0:T], in0=b[:, 0:T], in1=s[:, j, 0:T], op=ALU.mult),
                    [bRb, sgr_b[si]], [t2_b[i2]])
                dve(lambda e, n=n, i2=i2: e.tensor_tensor(out=mg[:, n, 0:T], in0=t1[i2][:, 0:T], in1=t2[i2][:, 0:T], op=ALU.add),
                    [t1_b[i2], t2_b[i2]], [mg_b[n]])
        g1 = modv(l, S, 2)
        for pn in range(4):
            slot, slb = next_slot()
            load_w(w_o[l], 0, KC, pn * 512, 512, slot, slb)
            for j in range(4):
                dch = pn * 4 + j
                bank, bb = next_bank()
                mm_group(bank, bb, (0, T), [(slot[:, k, j * 128:(j + 1) * 128], mg[:, k, 0:T]) for k in range(KC)], [slb] + mg_b)
                dve(lambda e, dch=dch, bank=bank: e.scalar_tensor_tensor(out=res[:, dch, 0:T], in0=bank[:, 0:T], scalar=g1[:, dch:dch + 1],
                                                                          in1=res[:, dch, 0:T], op0=ALU.mult, op1=ALU.add),
                    [bb, mod_b, res_b[dch]], [res_b[dch]])

    def swiglu_expert(T, h2, h2_b, Wg, Wu, Wd, nf, actT, actT_b, epilogue):
        sg = [cv_sg[0], cv_sg[1]]
        npan = nf // 4
        for fp in range(npan):
            sG, sGb = next_slot()
            load_w(Wg, 0, KC, fp * 512, 512, sG, sGb)
            sU, sUb = next_slot()
            load_w(Wu, 0, KC, fp * 512, 512, sU, sUb)
            for j in range(4):
                f = fp * 4 + j
                i2 = f % 2
                bG, bGb = next_bank()
                bU, bUb = next_bank()
                mm_group(bG, bGb, (0, T), [(sG[:, k, j * 128:(j + 1) * 128], h2[:, k, 0:T]) for k in range(KC)], [sGb] + h2_b)
                mm_group(bU, bUb, (0, T), [(sU[:, k, j * 128:(j + 1) * 128], h2[:, k, 0:T]) for k in range(KC)], [sUb] + h2_b)
                act_op(sg[i2][:, 0:T], bG[:, 0:T], AF.Silu, [bGb], [cv_sg_b[i2]])
                dve(lambda e, f=f, i2=i2, bU=bU: e.tensor_tensor(out=actT[:, f, 0:T], in0=bU[:, 0:T], in1=sg[i2][:, 0:T], op=ALU.mult),
                    [bUb, cv_sg_b[i2]], [actT_b[f]])
        slabs = []
        k0 = 0
        while k0 < nf:
            nk = min(14 if nf % 14 == 0 else 16, nf - k0)
            slabs.append((k0, nk))
            k0 += nk
        for dp in range(4):
            banks = [next_bank() for _ in range(4)]
            for si, (k0, nk) in enumerate(slabs):
                slot, slb = next_slot()
                load_w(Wd, k0, nk, dp * 512, 512, slot, slb)
                for j in range(4):
                    bank, bb = banks[j]
                    mm_group(bank, bb, (0, T), [(slot[:, k, j * 128:(j + 1) * 128], actT[:, k0 + k, 0:T]) for k in range(nk)],
                             [slb] + actT_b[k0:k0 + nk], first=(si == 0), last=(si == len(slabs) - 1))
            for j in range(4):
                epilogue(dp * 4 + j, banks[j][0], banks[j][1])

    cv_sg = [None, None]
    cv_sg_b = [Buf("sg0"), Buf("sg1")]

    def phaseD(l, S, ti, t0, T):
        cv.reset()
        last = (l == DEPTH - 1)
        moe = (l % 2 == 1)
        h2 = cv.take(KC * TT, BF16).rearrange("p (k t) -> p k t", k=KC)
        h2_b = [Buf("h2_%d" % k) for k in range(KC)]
        scr = norm_scratch()
        cv_sg[0] = cv.take(TT, F32)
        cv_sg[1] = cv.take(TT, F32)
        nf = (D_EXP if moe else D_FF) // 128
        actT = cv.take(nf * TT, BF16).rearrange("p (k t) -> p k t", k=nf)
        actT_b = [Buf("act%d" % k) for k in range(nf)]
        g2 = modv(l, S, 5)
        if not moe:
            norm_mod(T, drvA(l, S, 1), modv(l, S, 3), h2, h2_b, scr)

            def epi(dch, bank, bb):
                dve(lambda e, dch=dch, bank=bank: e.scalar_tensor_tensor(out=res[:, dch, 0:T], in0=bank[:, 0:T], scalar=g2[:, dch:dch + 1],
                                                                          in1=res[:, dch, 0:T], op0=ALU.mult, op1=ALU.add),
                    [bb, mod_b, res_b[dch]], [res_b[dch]])
            swiglu_expert(T, h2, h2_b, ffn_g[0], ffn_u[0], ffn_d[0], nf, actT, actT_b, epi)
        else:
            ntb = T // 128
            Bsb = cv.take(2 * TT, F32).rearrange("p (e t) -> p e t", e=2)
            Bsb_b = [Buf("Bsb0"), Buf("Bsb1")]
            h2f = scr[0]
            h2f_b = scr[1]
            diag = [cv.take(128, F32) for _ in range(2)]
            diag_b = [Buf("diag0"), Buf("diag1")]
            sm = cv.take(64, F32)
            sm_b = Buf("sm")
            comb = cv.take(ntb * NEXP, F32).rearrange("p (a e) -> p a e", a=ntb)
            comb_b = [Buf("comb%d" % a) for a in range(ntb)]
            tmp = scr[2]
            tmp_b = scr[3]
            lbanks = [next_bank() for _ in range(ntb)]
            Bv = modv(l, S, 3)

            def extra(k, xs_ap, xs_buf):
                i2 = k % 2
                act_op(h2f[i2][:, 0:T], xs_ap[:, 0:T], AF.Identity, [xs_buf, mod_b], [h2f_b[i2]], bias=Bv[:, k:k + 1])
                copy_op("dve", h2[:, k, 0:T], h2f[i2][:, 0:T], [h2f_b[i2]], [h2_b[k]])
                for a in range(ntb):
                    bank, bb = lbanks[a]
                    mm_group(bank, bb, (0, NEXP), [(h2f[i2][:, a * 128:(a + 1) * 128], router_sb[:, k, :])], [h2f_b[i2], rt_b],
                             first=(k == 0), last=(k == KC - 1))
            norm_mod(T, drvA(l, S, 1), None, h2, h2_b, scr, extra=extra)
            for a in range(ntb):
                bank, bb = lbanks[a]
                lg = sm[:, 0:8]
                eq1 = sm[:, 8:16]
                lg2 = sm[:, 16:24]
                eq2 = sm[:, 24:32]
                m1 = sm[:, 32:33]
                m2 = sm[:, 33:34]
                ee = sm[:, 34:35]
                g1_ = sm[:, 35:36]
                g2_ = sm[:, 36:37]
                c1 = sm[:, 40:48]
                R = [sm_b]
                dve(lambda e, bank=bank: e.tensor_copy(out=lg, in_=bank[:, 0:8]), [bb, sm_b], R)
                dve(lambda e: e.reduce_max(out=m1, in_=lg, axis=AX.X), R, R)
                dve(lambda e: e.tensor_scalar(out=eq1, in0=lg, scalar1=m1, scalar2=None, op0=ALU.is_equal), R, R)
                dve(lambda e: e.scalar_tensor_tensor(out=lg2, in0=eq1, scalar=-1e30, in1=lg, op0=ALU.mult, op1=ALU.add), R, R)
                dve(lambda e: e.reduce_max(out=m2, in_=lg2, axis=AX.X), R, R)
                dve(lambda e: e.tensor_scalar(out=eq2, in0=lg2, scalar1=m2, scalar2=None, op0=ALU.is_equal), R, R)
                dve(lambda e: e.tensor_tensor(out=ee, in0=m2, in1=m1, op=ALU.subtract), R, R)
                act_op(ee, ee, AF.Exp, R, R)
                dve(lambda e: e.tensor_scalar(out=g1_, in0=ee, scalar1=1.0, scalar2=None, op0=ALU.add), R, R)
                dve(lambda e: e.reciprocal(out=g1_, in_=g1_), R, R)
                dve(lambda e: e.tensor_tensor(out=g2_, in0=ee, in1=g1_, op=ALU.mult), R, R)
                dve(lambda e: e.tensor_scalar(out=c1, in0=eq1, scalar1=g1_, scalar2=None, op0=ALU.mult), R, R)
                dve(lambda e, a=a: e.scalar_tensor_tensor(out=comb[:, a, :], in0=eq2, scalar=g2_, in1=c1, op0=ALU.mult, op1=ALU.add),
                    R, [comb_b[a]])
            dctr = 0
            ectr2 = [0]
            for ex in range(NEXP):
                bank, bb = next_bank()
                bx = ex % 2
                for a in range(ntb):
                    i2 = dctr % 2
                    dctr += 1
                    dve(lambda e, i2=i2, a=a, ex=ex: e.tensor_scalar(out=diag[i2][:], in0=ident[:], scalar1=comb[:, a, ex:ex + 1], scalar2=None, op0=ALU.mult),
                        [comb_b[a], consts_b], [diag_b[i2]])
                    mm_group(bank, bb, (a * 128, (a + 1) * 128), [(ones[:], diag[i2][:])], [diag_b[i2], misc_b], fresh=(a == 0))
                copy_op("act", Bsb[:, bx, 0:T], bank[:, 0:T], [bb], [Bsb_b[bx]])

                def epi(dch, bank, bb, bx=bx):
                    i2 = ectr2[0] % 2
                    ectr2[0] += 1
                    dve(lambda e, dch=dch, bank=bank, i2=i2, bx=bx: e.scalar_tensor_tensor(out=tmp[i2][:, 0:T], in0=bank[:, 0:T], scalar=g2[:, dch:dch + 1],
                                                                                            in1=Bsb[:, bx, 0:T], op0=ALU.mult, op1=ALU.mult),
                        [bb, mod_b, Bsb_b[bx]], [tmp_b[i2]])
                    dve(lambda e, dch=dch, i2=i2: e.tensor_tensor(out=res[:, dch, 0:T], in0=res[:, dch, 0:T], in1=tmp[i2][:, 0:T], op=ALU.add),
                        [tmp_b[i2], res_b[dch]], [res_b[dch]])
                swiglu_expert(T, h2, h2_b, moe_g[0, ex], moe_u[0, ex], moe_d[0, ex], nf, actT, actT_b, epi)
        if not last:
            dma("sp", resT[S].rearrange("k p t -> p k t")[:, :, t0:t0 + T], res[:, :, 0:T], resT_b[S][ti],
                reads=res_b, writes=[resT_b[S][ti]])
        elif S == "lat":
            P.barrier()
            cv.reset()
            scr = norm_scratch()
            sq, sq_b, xs, xs_b, rstd, rstd_b = scr
            yf = cv.take(KC * TT, F32).rearrange("p (k t) -> p k t", k=KC)
            yf_b = [Buf("yf%d" % k) for k in range(KC)]
            otok = [cv.take(D, F32) for _ in range(2)]
            otok_b = [[Buf("otok%d_%d" % (i, q)) for q in range(4)] for i in range(2)]
            bank, bb = next_bank()
            for k in range(KC):
                i2 = k % 2
                act_op(sq[i2][:, 0:T], res[:, k, 0:T], AF.Square, [res_b[k]], [sq_b[i2]])
                mm_group(bank, bb, (0, T), [(ones[:], sq[i2][:, 0:T])], [sq_b[i2], misc_b], first=(k == 0), last=(k == KC - 1))
            act_op(rstd[:, 0:T], bank[:, 0:T], AF.Sqrt, [bb, misc_b], [rstd_b], bias=epsb[:], scale=1.0 / D)
            dve(lambda e: e.reciprocal(out=rstd[:, 0:T], in_=rstd[:, 0:T]), [rstd_b], [rstd_b])
            gfin = par[:, P_FIN:P_FIN + 16]
            for k in range(KC):
                dve(lambda e, k=k: e.scalar_tensor_tensor(out=yf[:, k, 0:T], in0=res[:, k, 0:T], scalar=gfin[:, k:k + 1], in1=rstd[:, 0:T],
                                                          op0=ALU.mult, op1=ALU.mult), [res_b[k], rstd_b, par_b], [yf_b[k]])
            for a in range(T // 128):
                oi = a % 2
                for q in range(4):
                    bank, bb = next_bank()

                    def fn(e, bank=bank, a=a, q=q):
                        ins = None
                        for kk in range(4):
                            k = q * 4 + kk
                            ins = e.transpose(bank[:, kk * 128:(kk + 1) * 128], yf[:, k, a * 128:(a + 1) * 128], ident[:])
                        return ins
                    P.op("pe", fn, reads=yf_b[q * 4:q * 4 + 4] + [consts_b], writes=[bb])
                    copy_op(ev_eng(), otok[oi][:, q * 512:(q + 1) * 512], bank[:, 0:512], [bb], [otok_b[oi][q]])
                dma("sp", out_d[t0 + a * 128:t0 + (a + 1) * 128, :], otok[oi][:], out_b, reads=otok_b[oi], pwrites=[out_b])

    stages = []

    def stage(name):
        stages.append(name)
        return stop_after is not None and len(stages) > stop_after

    def run():
        phase0()
        P.barrier()
        if stage("p0"):
            return
        for l in range(DEPTH):
            phase_ada(l)
        P.barrier()
        if dump:
            dma("sp", mod_dbg, mod[:], pb("moddbg"), reads=[mod_b])
        if stage("ada"):
            return
        for l in range(DEPTH):
            last = (l == DEPTH - 1)
            for S in ("ctx", "lat"):
                for ti, (t0, T) in enumerate(tile_list(S)):
                    phaseA(l, S, ti, t0, T, only_ur=(last and S == "ctx"))
                    P.barrier()
            if stage("A%d" % l):
                return
            phaseB(l)
            P.barrier()
            if stage("B%d" % l):
                return
            for S in ("ctx", "lat"):
                if last and S == "ctx":
                    continue
                phaseF(l, S)
                P.barrier()
            if stage("F%d" % l):
                return
            for S in ("ctx", "lat"):
                if last and S == "ctx":
                    continue
                for ti, (t0, T) in enumerate(tile_list(S)):
                    phaseC(l, S, ti, t0, T)
                    P.barrier()
                    phaseD(l, S, ti, t0, T)
                    P.barrier()
            if stage("CD%d" % l):
                return

    run()
    fin_reads = [out_b, zT_b] + [b for S in ("ctx", "lat") for b in resT_b[S] + proj_b[S] + YT_b[S]] + [Pd_b["ctx"], Pd_b["lat"]]
    P.op("sp", None, reads=fin_reads, barrier=True)

    all_bufs_with_dma = set()
    for e in P.ENGS:
        for o in P.q[e]:
            if o.dma:
                all_bufs_with_dma.add(o.dst)
    for i, b in enumerate(sorted(all_bufs_with_dma, key=lambda b: b.name)):
        b.dsem = es.enter_context(nc.semaphore("d%d_%s" % (i, b.name)))
    sems = {e: es.enter_context(nc.semaphore("eng_" + e)) for e in P.ENGS}
    with nc.Block() as block:
        @block.tensor
        def _(e):
            P.emit_one("pe", e, sems)

        @block.scalar
        def _(e):
            P.emit_one("act", e, sems)

        @block.vector
        def _(e):
            P.emit_one("dve", e, sems)

        @block.gpsimd
        def _(e):
            P.emit_one("pool", e, sems)

        @block.sync
        def _(e):
            P.emit_one("sp", e, sems)
    es.close()
    return nc, stages


def _prog_mark(self):
    for e in self.ENGS:
        for o in self.q[e]:
            for d in o.raw + o.war:
                if not d.dma:
                    d.sig = True
    for e in self.ENGS:
        n = 0
        for o in self.q[e]:
            if o.sig and not o.dma:
                n += 1
                o.idx = n
    self.marked = True


def _prog_emit_one(self, e, eng, sems):
    if not getattr(self, "marked", False):
        _prog_mark(self)
    waited = {}

    def need(sem, val):
        if waited.get(sem.name, 0) < val:
            eng.wait_ge(sem, val)
            waited[sem.name] = val

    for o in self.q[e]:
        for lst, is_war in ((o.raw, False), (o.war, True)):
            for d in lst:
                if d.dma:
                    need(d.dst.dsem, d.dval)
                else:
                    if d.eng == e and (e in ("pe", "sp", "pool") or is_war):
                        continue
                    need(sems[d.eng], d.idx)
        ins = o.fn(eng) if o.fn is not None else None
        if o.dma:
            ins.then_inc(o.dst.dsem, 16)
        elif o.sig:
            if ins is None:
                ins = eng.nop()
            ins.then_inc(sems[e], 1)


Prog.emit_one = _prog_emit_one


def _host_consts():
    j = np.arange(256)
    ang = 2.0 * np.pi * ((j[:, None] * j[None, :]) % 256) / 256.0
    cs256 = np.concatenate([np.cos(ang), np.sin(ang)], axis=1) / 16.0

    def tp(L):
        t = np.arange(L)
        a = 2.0 * np.pi * ((t[:, None] * t[None, :]) % L) / float(L)
        return np.stack([np.cos(a), -np.sin(a)], axis=0) / np.sqrt(float(L))
    return (np.ascontiguousarray(cs256, dtype=np.float32), np.ascontiguousarray(tp(SEQ), dtype=np.float32),
            np.ascontiguousarray(tp(CTX), dtype=np.float32), np.eye(128, dtype=np.float32))


def _fm(vec, nchunk):
    return np.ascontiguousarray(np.asarray(vec, dtype=np.float32).reshape(nchunk, 128).T)


def _params(inp):
    par = np.zeros((128, NPAR), np.float32)
    for l in range(DEPTH):
        b = l * P_LSZ
        par[:, b + P_ADAB:b + P_ADAB + 96] = _fm(inp["ada_b"][l], 96)
        par[:, b + P_N1:b + P_N1 + 16] = _fm(inp["norm1_g"][l], 16)
        par[:, b + P_N2:b + P_N2 + 16] = _fm(inp["norm2_g"][l], 16)
        for k in range(4):
            par[:, b + P_CW + k * 8:b + P_CW + k * 8 + 8] = _fm(inp["conv_w"][l, k], 8)
        par[:, b + P_CB:b + P_CB + 8] = _fm(inp["conv_b"][l], 8)
        for d in range(2):
            par[:, b + P_BA + d * 8:b + P_BA + d * 8 + 8] = _fm(inp["lru_ba"][l, d], 8)
            par[:, b + P_BX + d * 8:b + P_BX + d * 8 + 8] = _fm(inp["lru_bx"][l, d], 8)
            par[:, b + P_LAM + d * 8:b + P_LAM + d * 8 + 8] = _fm(inp["lru_lambda"][l, d], 8)
    par[:, P_FIN:P_FIN + 16] = _fm(inp["final_norm_g"], 16)
    return par


_CACHE = {}


def make_in_maps(inp):
    cs256, tpl, tpc, ident = _host_consts()
    par = _params(inp)
    shared = {"params": par, "ident": ident, "cs256": cs256, "tp_lat": tpl, "tp_ctx": tpc}
    for name in ("ada_w", "w_in", "lru_wa", "lru_wx", "w_fourier_out", "w_lru_out", "w_out", "ffn_w_gate", "ffn_w_up",
                 "ffn_w_down", "moe_router", "moe_w_gate", "moe_w_up", "moe_w_down"):
        shared[name] = np.ascontiguousarray(np.asarray(inp[name], dtype=np.float32))
    maps = []
    for b in range(8):
        m = dict(shared)
        m["x"] = np.ascontiguousarray(np.asarray(inp["x"][b], dtype=np.float32))
        m["ctx"] = np.ascontiguousarray(np.asarray(inp["ctx"][b], dtype=np.float32))
        cc = np.stack([_fm(inp["c"][b], KC), _fm(inp["c_ctx"], KC)], axis=-1)
        m["cc"] = np.ascontiguousarray(cc, dtype=np.float32)
        maps.append(m)
    return maps


def kernel(**inputs):
    if "nc" not in _CACHE:
        _CACHE["nc"] = build_program()[0]
    nc = _CACHE["nc"]
    in_maps = make_in_maps(inputs)
    r = run_bass_kernel_spmd(nc, in_maps, core_ids=list(range(8)))
    return np.stack([np.asarray(r.results[b]["out"], dtype=np.float32) for b in range(8)], axis=0)
```

```python
import numpy as np
import concourse.bass as bass
import concourse.mybir as mybir
from concourse.bass_utils import run_bass_kernel_spmd

F32 = mybir.dt.float32
BF16 = mybir.dt.bfloat16
U8 = mybir.dt.uint8
AF = mybir.ActivationFunctionType
ALU = mybir.AluOpType
AX = mybir.AxisListType

D = 2048
KC = 16
SEQ = 2048
CTX = 256
NTOK = CTX + SEQ
DEPTH = 2
N_IN = 7168
D_FF = 5632
D_EXP = 7168
NEXP = 8
EPS = 1e-6
TT = 512

P_ADAB = 0
P_N1 = 96
P_N2 = 112
P_CW = 128
P_CB = 160
P_BA = 168
P_BX = 184
P_LAM = 200
P_LSZ = 216
P_FIN = DEPTH * P_LSZ
NPAR = P_FIN + 16

DEBUG = {"stop_after": None, "dump": False}


class Buf:
    __slots__ = ("name", "writers", "readers", "prev_readers", "dsem", "dcount")

    def __init__(self, name):
        self.name = name
        self.writers = []
        self.readers = []
        self.prev_readers = []
        self.dsem = None
        self.dcount = 0


class Op:
    __slots__ = ("eng", "fn", "raw", "war", "dma", "dst", "dval", "sig", "idx")

    def __init__(self, eng, fn):
        self.eng = eng
        self.fn = fn
        self.raw = []
        self.war = []
        self.dma = False
        self.dst = None
        self.dval = 0
        self.sig = False
        self.idx = 0


class Prog:
    ENGS = ("pe", "act", "dve", "pool", "sp")

    def __init__(self, nc):
        self.nc = nc
        self.q = {e: [] for e in self.ENGS}
        self.bar = Buf("BAR")
        self.nops = 0

    @staticmethod
    def _add_reader(lst, o):
        if not o.dma:
            for i, r in enumerate(lst):
                if (not r.dma) and r.eng == o.eng:
                    lst[i] = o
                    return
        lst.append(o)

    def op(self, eng, fn, reads=(), writes=(), pwrites=(), dma_dst=None, barrier=False, nobar=False):
        o = Op(eng, fn)
        if dma_dst is not None:
            o.dma = True
            o.dst = dma_dst
            dma_dst.dcount += 1
            o.dval = 16 * dma_dst.dcount
        rd = list(reads)
        wr = list(writes)
        if barrier:
            wr.append(self.bar)
        elif not nobar:
            rd.append(self.bar)
        for b in rd:
            o.raw.extend(b.writers)
            self._add_reader(b.readers, o)
        for b in wr:
            o.raw.extend(b.writers)
            if b is self.bar:
                o.raw.extend(b.readers)
            else:
                o.war.extend(b.readers)
            o.war.extend(b.prev_readers)
            b.writers = [o]
            b.readers = []
            b.prev_readers = []
        for b in pwrites:
            o.war.extend(b.readers)
            o.war.extend(b.prev_readers)
            if b.readers:
                b.prev_readers = b.readers
                b.readers = []
                b.writers = []
            self._add_reader(b.writers, o)
        self.q[eng].append(o)
        self.nops += 1
        return o

    def barrier(self):
        self.op("dve", None, barrier=True)

    def emit(self, block_engines, sems):
        for e in self.ENGS:
            for o in self.q[e]:
                for d in o.raw + o.war:
                    if not d.dma:
                        d.sig = True
        for e in self.ENGS:
            n = 0
            for o in self.q[e]:
                if o.sig and not o.dma:
                    n += 1
                    o.idx = n
        for e in self.ENGS:
            eng = block_engines[e]
            waited = {}
            pending_inc = [0]

            def need(sem, val, waited=waited, eng=eng):
                if waited.get(sem.name, 0) < val:
                    eng.wait_ge(sem, val)
                    waited[sem.name] = val

            for o in self.q[e]:
                for d, is_war in [(x, False) for x in o.raw] + [(x, True) for x in o.war]:
                    if d.dma:
                        need(d.dst.dsem, d.dval)
                    else:
                        if d.eng == e:
                            if e in ("pe", "sp", "pool") or is_war:
                                continue
                        need(sems[d.eng], d.idx)
                ins = o.fn(eng) if o.fn is not None else None
                if o.dma:
                    ins.then_inc(o.dst.dsem, 16)
                elif o.sig:
                    if ins is None:
                        ins = eng.nop()
                    ins.then_inc(sems[e], 1)


def build_program(stop_after=None, dump=False):
    nc = bass.Bass("TRN2", target_bir_lowering=False)
    from contextlib import ExitStack
    es = ExitStack()

    def din(name, shape, dt=F32):
        return nc.dram_tensor(name, list(shape), dt, kind="ExternalInput").ap()

    def dscr(name, shape, dt=F32):
        return nc.dram_tensor(name, list(shape), dt, kind=("ExternalOutput" if dump else "Internal")).ap()

    x_d = din("x", [SEQ, D])
    ctx_d = din("ctx", [CTX, D])
    cc_d = din("cc", [128, KC, 2])
    par_d = din("params", [128, NPAR])
    ident_d = din("ident", [128, 128])
    cs_d = din("cs256", [256, 512])
    tpl_d = din("tp_lat", [2, SEQ, SEQ])
    tpc_d = din("tp_ctx", [2, CTX, CTX])
    ada_w = din("ada_w", [DEPTH, D, 6 * D])
    w_in = din("w_in", [DEPTH, D, N_IN])
    lru_wa = din("lru_wa", [DEPTH, 2, 8, 128, 128])
    lru_wx = din("lru_wx", [DEPTH, 2, 8, 128, 128])
    w_fo = din("w_fourier_out", [DEPTH, 1024, D])
    w_ro = din("w_lru_out", [DEPTH, 1024, D])
    w_o = din("w_out", [DEPTH, D, D])
    ffn_g = din("ffn_w_gate", [1, D, D_FF])
    ffn_u = din("ffn_w_up", [1, D, D_FF])
    ffn_d = din("ffn_w_down", [1, D_FF, D])
    router_d = din("moe_router", [1, D, NEXP])
    moe_g = din("moe_w_gate", [1, NEXP, D, D_EXP])
    moe_u = din("moe_w_up", [1, NEXP, D, D_EXP])
    moe_d = din("moe_w_down", [1, NEXP, D_EXP, D])
    out_d = nc.dram_tensor("out", [SEQ, D], F32, kind="ExternalOutput").ap()

    resT = {"lat": dscr("resT_lat", [KC, 128, SEQ]), "ctx": dscr("resT_ctx", [KC, 128, CTX])}
    urT = dscr("urT", [8, 128, NTOK])
    ggT = dscr("ggT", [8, 128, NTOK])
    sgfT = dscr("sgfT", [KC, 128, NTOK])
    sgrT = dscr("sgrT", [KC, 128, NTOK])
    Pd = {"lat": dscr("P_lat", [SEQ, 2, 1024], BF16), "ctx": dscr("P_ctx", [CTX, 2, 1024], BF16)}
    YTd = dscr("YT", [8, 128, NTOK], BF16)
    zTd = dscr("zT", [8, 128, NTOK], BF16)
    mod_dbg = dscr("mod_dbg", [128, DEPTH * 2 * 96]) if dump else None

    P = Prog(nc)

    def sb(name, shape, dt):
        return es.enter_context(nc.sbuf_tensor("s_" + name, list(shape), dt))

    WSLOTS = 4
    wslot = [sb("wslot%d" % i, [128, 16, 512], BF16) for i in range(WSLOTS)]
    wslot_b = [Buf("wslot%d" % i) for i in range(WSLOTS)]
    wctr = [0]

    def next_slot():
        i = wctr[0] % WSLOTS
        wctr[0] += 1
        return wslot[i], wslot_b[i]

    res = sb("res", [128, KC, TT], F32)
    res_b = [Buf("res%d" % k) for k in range(KC)]
    ident = sb("ident", [128, 128], F32)
    ones = sb("ones", [128, 128], F32)
    cs_sb = sb("cs_sb", [128, 2, 512], BF16)
    par = sb("par", [128, NPAR], F32)
    mod = sb("mod", [128, DEPTH * 2 * 96], F32)
    drv = sb("drv", [128, DEPTH * 2 * 64], F32)
    s8 = sb("s8", [128, DEPTH * 32], F32)
    cc_sb = sb("cc_sb", [128, KC, 2], F32)
    sc_bf = sb("sc_bf", [128, KC, 2], BF16)
    router_sb = sb("router_sb", [128, KC, NEXP], F32)
    epsb = sb("epsb", [128, 1], F32)
    oneb = sb("oneb", [128, 1], F32)
    ARENA = 100 * 1024
    arena = sb("arena", [128, ARENA], U8)
    consts_b = Buf("consts")
    par_b = Buf("par")
    mod_b = Buf("mod")
    drv_b = Buf("drv")

    class Carve:
        def __init__(self):
            self.off = 0

        def reset(self):
            self.off = 0

        def take(self, nelem, dt, shape=None):
            esz = 4 if dt == F32 else 2
            nb = nelem * esz
            nb = (nb + 63) // 64 * 64
            assert self.off + nb <= ARENA, ("arena overflow", self.off, nb)
            a = arena[:, self.off:self.off + nelem * esz].bitcast(dt)
            self.off += nb
            return a

    cv = Carve()
    _pb = {}

    def pb(name):
        if name not in _pb:
            _pb[name] = Buf(name)
        return _pb[name]

    psum = [es.enter_context(nc.psum_tensor("ps%d" % i, [128, 512], F32)) for i in range(8)]
    psum_b = [Buf("ps%d" % i) for i in range(8)]
    pctr = [0]

    def next_bank():
        i = pctr[0] % 8
        pctr[0] += 1
        return psum[i], psum_b[i]

    ectr = [0]

    def ev_eng():
        ectr[0] += 1
        return "act" if ectr[0] % 2 else "dve"

    def dma(eng, out_ap, in_ap, dst_buf, reads=(), writes=(), pwrites=(), nobar=False):
        fn = lambda e, o=out_ap, i=in_ap: e.dma_start(out=o, in_=i)
        return P.op(eng, fn, reads=reads, writes=writes, pwrites=pwrites, dma_dst=dst_buf, nobar=nobar)

    def load_w(dram_ap_2d, k0, nk, c0, ncol, slot, slot_buf, kc_off=0, first=True):
        src = dram_ap_2d[k0 * 128:(k0 + nk) * 128, c0:c0 + ncol].rearrange("(k p) n -> p k n", p=128)
        dst = slot[:, kc_off:kc_off + nk, 0:ncol]
        if first:
            return dma("pool", dst, src, slot_buf, writes=[slot_buf], nobar=True)
        return dma("pool", dst, src, slot_buf, pwrites=[slot_buf], nobar=True)

    def mm_group(bank, bank_b, cols, pairs, reads, first=True, last=True, fresh=None):
        if fresh is None:
            fresh = first
        def fn(e, bank=bank, cols=cols, pairs=pairs, first=first, last=last):
            n = len(pairs)
            ins = None
            for i, (l, r) in enumerate(pairs):
                ins = e.matmul(bank[:, cols[0]:cols[1]], l, r, start=(first and i == 0), stop=(last and i == n - 1))
            return ins
        if fresh:
            return P.op("pe", fn, reads=reads, writes=[bank_b])
        return P.op("pe", fn, reads=reads, pwrites=[bank_b])

    def act_op(out_ap, in_ap, func, reads, writes, bias=None, scale=None, eng="act"):
        kw = {}
        if bias is not None:
            kw["bias"] = bias
        if scale is not None:
            kw["scale"] = scale
        return P.op("act", lambda e, o=out_ap, i=in_ap, f=func, kw=kw: e.activation(out=o, in_=i, func=f, **kw),
                    reads=reads, writes=writes)

    def copy_op(eng, out_ap, in_ap, reads, writes):
        if eng == "act":
            return P.op("act", lambda e, o=out_ap, i=in_ap: e.activation(out=o, in_=i, func=AF.Copy), reads=reads, writes=writes)
        return P.op("dve", lambda e, o=out_ap, i=in_ap: e.tensor_copy(out=o, in_=i), reads=reads, writes=writes)

    def dve(fn, reads, writes):
        return P.op("dve", fn, reads=reads, writes=writes)

    dma("sp", ident[:], ident_d, consts_b, writes=[consts_b])
    dma("sp", par[:], par_d, par_b, writes=[par_b])
    cc_b = Buf("cc")
    dma("sp", cc_sb[:], cc_d, cc_b, writes=[cc_b])
    rt_b = Buf("router")
    dma("sp", router_sb[:], router_d[0].rearrange("(k p) e -> p k e", p=128), rt_b, writes=[rt_b])
    csb_b = Buf("cs")
    dma("pool", cs_sb[:], cs_d.rearrange("(c p) n -> p c n", p=128), csb_b, writes=[csb_b])
    misc_b = Buf("misc")
    dve(lambda e: e.memset(ones[:], 1.0), [], [misc_b])
    dve(lambda e: e.memset(epsb[:], EPS), [], [misc_b])
    dve(lambda e: e.memset(oneb[:], 1.0), [], [misc_b])

    def tile_list(S):
        if S == "ctx":
            return [(0, CTX)]
        return [(t * TT, TT) for t in range(SEQ // TT)]

    COL0 = {"ctx": 0, "lat": CTX}
    SIDX = {"lat": 0, "ctx": 1}
    resT_b = {S: [Buf("resT_%s%d" % (S, i)) for i in range(len(tile_list(S)))] for S in ("ctx", "lat")}
    proj_b = {S: [Buf("proj_%s%d" % (S, i)) for i in range(len(tile_list(S)))] for S in ("ctx", "lat")}
    Pd_b = {S: Buf("Pd_%s" % S) for S in ("ctx", "lat")}
    YT_b = {S: [Buf("YT_%s%d" % (S, i)) for i in range(len(tile_list(S)))] for S in ("ctx", "lat")}
    zT_b = Buf("zT")
    out_b = Buf("out")

    def phase0(hook=None):
        cv.reset()
        xin = cv.take(4 * D, F32).rearrange("p (a f) -> p a f", a=4)
        xin_b = pb("xin")
        for S, src in (("ctx", ctx_d), ("lat", x_d)):
            for ti, (t0, T) in enumerate(tile_list(S)):
                ntb = T // 128
                dma("sp", xin[:, 0:ntb, :], src[t0:t0 + T, :].rearrange("(a p) f -> p a f", p=128), xin_b, writes=[xin_b])
                for k in range(KC):
                    bank, bb = next_bank()

                    def fn(e, bank=bank, k=k, ntb=ntb):
                        ins = None
                        for tb in range(ntb):
                            ins = e.transpose(bank[:, tb * 128:(tb + 1) * 128], xin[:, tb, k * 128:(k + 1) * 128], ident[:])
                        return ins
                    P.op("pe", fn, reads=[xin_b, consts_b], writes=[bb])
                    copy_op(ev_eng(), res[:, k, 0:T], bank[:, 0:T], [bb], [res_b[k]])
                dma("sp", resT[S].rearrange("k p t -> p k t")[:, :, t0:t0 + T], res[:, :, 0:T], resT_b[S][ti],
                    reads=res_b, writes=[resT_b[S][ti]])
                if hook is not None:
                    hook()

    def ada_steps(l):
        sc_b = Buf("sc_bf%d" % l)
        adab = par[:, l * P_LSZ + P_ADAB:l * P_LSZ + P_ADAB + 96]
        modl = mod[:, l * 192:(l + 1) * 192].rearrange("p (s n) -> p s n", s=2)
        steps = []

        def start():
            act_op(sc_bf[:], cc_sb[:], AF.Silu, [cc_b], [sc_b])

        def panel(pn):
            slot, slb = next_slot()
            load_w(ada_w[l], 0, KC, pn * 512, 512, slot, slb)
            bank, bb = next_bank()
            for j in range(4):
                pairs = [(slot[:, k, j * 128:(j + 1) * 128], sc_bf[:, k, :]) for k in range(KC)]
                mm_group(bank, bb, (2 * j, 2 * j + 2), pairs, [slb, sc_b], fresh=(j == 0))
            for j in range(4):
                n = pn * 4 + j
                P.op("dve", lambda e, o=modl[:, :, n], i=bank[:, 2 * j:2 * j + 2], s=adab[:, n:n + 1]:
                     e.tensor_scalar(out=o, in0=i, scalar1=s, scalar2=None, op0=ALU.add),
                     reads=[bb, par_b], pwrites=[mod_b])

        def finish():
            for s_ in range(2):
                for which, (goff, scoff) in enumerate(((P_N1, 16), (P_N2, 64))):
                    o = drv[:, (l * 2 + s_) * 64 + which * 16:(l * 2 + s_) * 64 + which * 16 + 16]
                    scv = modl[:, s_, scoff:scoff + 16]
                    g = par[:, l * P_LSZ + goff:l * P_LSZ + goff + 16]
                    P.op("dve", lambda e, o=o, scv=scv, g=g: e.scalar_tensor_tensor(out=o, in0=scv, scalar=1.0, in1=g, op0=ALU.add, op1=ALU.mult),
                         reads=[mod_b, par_b], pwrites=[drv_b])
            lam = par[:, l * P_LSZ + P_LAM:l * P_LSZ + P_LAM + 16]
            t = drv[:, (l * 2) * 64 + 32:(l * 2) * 64 + 48]
            u = drv[:, (l * 2) * 64 + 48:(l * 2) * 64 + 64]
            t2 = drv[:, (l * 2 + 1) * 64 + 32:(l * 2 + 1) * 64 + 48]
            t3 = drv[:, (l * 2 + 1) * 64 + 48:(l * 2 + 1) * 64 + 64]
            tb_ = Buf("s8tmp%d" % l)
            s8_b = s8_bufs[l]
            act_op(t, lam, AF.Exp, [par_b], [tb_], scale=-1.0)
            dve(lambda e: e.tensor_scalar(out=u, in0=t, scalar1=1.0, scalar2=None, op0=ALU.add), [tb_], [tb_])
            dve(lambda e: e.tensor_scalar(out=t2, in0=u, scalar1=-1.0, scalar2=1e-30, op0=ALU.add, op1=ALU.max), [tb_], [tb_])
            dve(lambda e: e.reciprocal(out=t2, in_=t2), [tb_], [tb_])
            act_op(t3, u, AF.Ln, [tb_], [tb_])
            dve(lambda e: e.tensor_tensor(out=t3, in0=t3, in1=t, op=ALU.mult), [tb_], [tb_])
            dve(lambda e: e.scalar_tensor_tensor(out=s8[:, l * 32:l * 32 + 16], in0=t3, scalar=-8.0, in1=t2, op0=ALU.mult, op1=ALU.mult), [tb_], [s8_b])
            dve(lambda e: e.tensor_scalar(out=s8[:, l * 32 + 16:l * 32 + 32], in0=s8[:, l * 32:l * 32 + 16], scalar1=2.0, scalar2=None, op0=ALU.mult), [s8_b], [s8_b])

        steps.append(start)
        for pn in range(24):
            steps.append(lambda pn=pn: panel(pn))
        steps.append(finish)
        return steps

    def run_steps(steps, n):
        for _ in range(n):
            if steps:
                steps.pop(0)()

    s8_bufs = [Buf("s8_%d" % l) for l in range(DEPTH)]

    def modv(l, S, idx):
        base = l * 192 + SIDX[S] * 96 + idx * 16
        return mod[:, base:base + 16]

    def drvA(l, S, which):
        base = (l * 2 + SIDX[S]) * 64 + which * 16
        return drv[:, base:base + 16]

    def norm_mod(T, Avec, Bvec, hT, hT_b, scratch, extra=None):
        sq, sq_b, xs, xs_b, rstd, rstd_b = scratch
        bank, bb = next_bank()
        for k in range(KC):
            i2 = k % 2
            act_op(sq[i2][:, 0:T], res[:, k, 0:T], AF.Square, [res_b[k]], [sq_b[i2]])
            mm_group(bank, bb, (0, T), [(ones[:], sq[i2][:, 0:T])], [sq_b[i2], misc_b], first=(k == 0), last=(k == KC - 1))
        act_op(rstd[:, 0:T], bank[:, 0:T], AF.Sqrt, [bb, misc_b], [rstd_b], bias=epsb[:], scale=1.0 / D)
        dve(lambda e: e.reciprocal(out=rstd[:, 0:T], in_=rstd[:, 0:T]), [rstd_b], [rstd_b])
        for k in range(KC):
            i2 = k % 2
            dve(lambda e, k=k, i2=i2: e.scalar_tensor_tensor(out=xs[i2][:, 0:T], in0=res[:, k, 0:T], scalar=Avec[:, k:k + 1],
                                                               in1=rstd[:, 0:T], op0=ALU.mult, op1=ALU.mult),
                [res_b[k], rstd_b, drv_b, par_b], [xs_b[i2]])
            if Bvec is not None:
                act_op(hT[:, k, 0:T], xs[i2][:, 0:T], AF.Identity, [xs_b[i2], mod_b], [hT_b[k]], bias=Bvec[:, k:k + 1])
            if extra is not None:
                extra(k, xs[i2], xs_b[i2])

    def norm_scratch():
        sq = [cv.take(TT, F32) for _ in range(2)]
        xs = [cv.take(TT, F32) for _ in range(2)]
        rstd = cv.take(TT, F32)
        return (sq, [Buf("sq0"), Buf("sq1")], xs, [Buf("xs0"), Buf("xs1")], rstd, Buf("rstd"))

    def phaseA_all(l):
        cv.reset()
        last = (l == DEPTH - 1)
        hTs = [cv.take(KC * TT, BF16).rearrange("p (k t) -> p k t", k=KC) for _ in range(2)]
        hTs_b = [[Buf("hT%d_%d" % (i, k)) for k in range(KC)] for i in range(2)]
        scr = norm_scratch()
        ufT = cv.take(8 * TT, BF16).rearrange("p (k t) -> p k t", k=8)
        ufT_b = [Buf("ufT%d" % k) for k in range(8)]
        Pt = cv.take(4 * 2048, BF16).rearrange("p (a c j) -> p a c j", a=4, c=2)
        Pt_b = Buf("Pt")
        stage = [cv.take(4 * TT, F32).rearrange("p (j t) -> p j t", j=4) for _ in range(2)]
        stage_b = [[Buf("st%d_%d" % (i, j)) for j in range(4)] for i in range(2)]
        tiles = []
        for S in ("ctx", "lat"):
            for ti, (t0, T) in enumerate(tile_list(S)):
                tiles.append((S, ti, t0, T, last and S == "ctx"))

        def prep(i):
            S, ti, t0, T, only_ur = tiles[i]
            dma("sp", res[:, :, 0:T], resT[S].rearrange("k p t -> p k t")[:, :, t0:t0 + T], res_b[0],
                reads=[resT_b[S][ti]], writes=res_b, nobar=True)
            norm_mod(T, drvA(l, S, 0), modv(l, S, 0), hTs[i % 2], hTs_b[i % 2], scr)

        sctr = [0]
        prep(0)
        for i, (S, ti, t0, T, only_ur) in enumerate(tiles):
            hT, hT_b = hTs[i % 2], hTs_b[i % 2]
            c0 = COL0[S] + t0
            panels = [2, 3] if only_ur else list(range(14))
            mid = len(panels) // 2
            for pi, pn in enumerate(panels):
                if pi == mid and i + 1 < len(tiles):
                    prep(i + 1)
                slot, slb = next_slot()
                load_w(w_in[l], 0, KC, pn * 512, 512, slot, slb)
                seg = pn // 2 if pn < 6 else (3 if pn < 10 else 4)
                sti = sctr[0] % 2
                for j in range(4):
                    n = pn * 4 + j
                    bank, bb = next_bank()
                    pairs = [(slot[:, k, j * 128:(j + 1) * 128], hT[:, k, 0:T]) for k in range(KC)]
                    mm_group(bank, bb, (0, T), pairs, [slb] + hT_b)
                    if seg == 0:
                        copy_op(ev_eng(), ufT[:, n, 0:T], bank[:, 0:T], [bb], [ufT_b[n]])
                    elif seg == 1:
                        copy_op("dve", stage[sti][:, j, 0:T], bank[:, 0:T], [bb], [stage_b[sti][j]])
                    elif seg == 2:
                        act_op(stage[sti][:, j, 0:T], bank[:, 0:T], AF.Gelu_apprx_tanh, [bb], [stage_b[sti][j]])
                    else:
                        act_op(stage[sti][:, j, 0:T], bank[:, 0:T], AF.Sigmoid, [bb], [stage_b[sti][j]])
                if seg == 0:
                    if pn == 1:
                        for tb in range(T // 128):
                            for g in range(4):
                                bank, bb = next_bank()
                                pairs = [(ufT[:, 2 * g + c2, tb * 128:(tb + 1) * 128], cs_sb[:, c2, :]) for c2 in range(2)]
                                mm_group(bank, bb, (0, 512), pairs, [ufT_b[2 * g], ufT_b[2 * g + 1], csb_b])
                                o = Pt[:, tb, :, g * 256:(g + 1) * 256]
                                iap = bank[:, 0:512].rearrange("p (c j) -> p c j", c=2)
                                eng = ev_eng()
                                if eng == "act":
                                    P.op("act", lambda e, o=o, i=iap: e.activation(out=o, in_=i, func=AF.Copy), reads=[bb], pwrites=[Pt_b])
                                else:
                                    P.op("dve", lambda e, o=o, i=iap: e.tensor_copy(out=o, in_=i), reads=[bb], pwrites=[Pt_b])
                        dma("sp", Pd[S][t0:t0 + T].rearrange("(a p) c j -> p a c j", p=128), Pt[:, 0:T // 128], Pd_b[S],
                            reads=[Pt_b], pwrites=[Pd_b[S]])
                else:
                    if seg == 1:
                        dst = urT[(pn - 2) * 4:(pn - 2) * 4 + 4]
                    elif seg == 2:
                        dst = ggT[(pn - 4) * 4:(pn - 4) * 4 + 4]
                    elif seg == 3:
                        dst = sgfT[(pn - 6) * 4:(pn - 6) * 4 + 4]
                    else:
                        dst = sgrT[(pn - 10) * 4:(pn - 10) * 4 + 4]
                    dma("sp", dst.rearrange("k p t -> p k t")[:, :, c0:c0 + T], stage[sti][:, :, 0:T], proj_b[S][ti],
                        reads=stage_b[sti], pwrites=[proj_b[S][ti]])
                    sctr[0] += 1

    def phaseB(l, hook=None):
        cv.reset()
        N = NTOK
        ur = cv.take(N, F32)
        gg = cv.take(N, F32)
        v = cv.take(N, F32)
        vbf = cv.take(N, BF16)
        r_ = cv.take(N, F32)
        i_ = cv.take(N, F32)
        a_ = cv.take(N, F32)
        m_ = cv.take(N, F32)
        hf = cv.take(N, F32)
        zst = cv.take(N, BF16)
        gwall = cv.take(8 * 4 * 128, BF16).rearrange("p (c g j) -> p c g j", c=8, g=4)
        v_b, vbf_b, r_b, i_b, a_b, m_b, hf_b, zst_b = [Buf(n) for n in ("v", "vbf", "r", "i", "a", "m", "hf", "zst")]
        ur_b, gg_b, gw_b = pb("ur"), pb("gg"), pb("gw")
        allproj = proj_b["ctx"] + proj_b["lat"]
        lb = l * P_LSZ
        blocks = [(0, CTX)] + [(CTX + t * TT, TT) for t in range(SEQ // TT)]
        for d_ in range(2):
            if d_ == 0:
                dma("pool", gwall[:, :, d_, :], lru_wa[l, d_].rearrange("c i j -> i c j"), gw_b, writes=[gw_b])
            else:
                dma("pool", gwall[:, :, d_, :], lru_wa[l, d_].rearrange("c i j -> i c j"), gw_b, pwrites=[gw_b])
            dma("pool", gwall[:, :, 2 + d_, :], lru_wx[l, d_].rearrange("c i j -> i c j"), gw_b, pwrites=[gw_b])
        for c in range(8):
            gw = gwall[:, c]
            dma("sp", ur[:], urT[c], ur_b, reads=allproj, writes=[ur_b])
            dma("sp", gg[:], ggT[c], gg_b, reads=allproj, writes=[gg_b])
            cwl = [par[:, lb + P_CW + k * 8 + c:lb + P_CW + k * 8 + c + 1] for k in range(4)]
            cw = lambda k, cwl=cwl: cwl[k]
            cb = par[:, lb + P_CB + c:lb + P_CB + c + 1]
            dve(lambda e, cw=cw, cb=cb: e.tensor_scalar(out=v[:], in0=ur[:], scalar1=cw(2), scalar2=cb, op0=ALU.mult, op1=ALU.add),
                [ur_b, par_b], [v_b])
            for k in (0, 1, 3):
                off = k - 2
                lo = max(0, -off)
                hi = max(0, off)
                dve(lambda e, cw=cw, k=k, lo=lo, hi=hi, off=off: e.scalar_tensor_tensor(
                    out=v[:, lo:CTX - hi], in0=ur[:, lo + off:CTX - hi + off], scalar=cw(k), in1=v[:, lo:CTX - hi],
                    op0=ALU.mult, op1=ALU.add), [ur_b, par_b, v_b], [v_b])
                v3 = v[:, CTX:N].rearrange("p (r w) -> p r w", w=64)
                u3 = ur[:, CTX:N].rearrange("p (r w) -> p r w", w=64)
                dve(lambda e, cw=cw, k=k, lo=lo, hi=hi, off=off, v3=v3, u3=u3: e.scalar_tensor_tensor(
                    out=v3[:, :, lo:64 - hi], in0=u3[:, :, lo + off:64 - hi + off], scalar=cw(k), in1=v3[:, :, lo:64 - hi],
                    op0=ALU.mult, op1=ALU.add), [ur_b, par_b, v_b], [v_b])
            copy_op("act", vbf[:], v[:], [v_b], [vbf_b])
            hb = None
            for d in range(2):
                for gi, (dst, dst_b, boff) in enumerate(((r_, r_b, P_BA), (i_, i_b, P_BX))):
                    bias = par[:, lb + boff + d * 8 + c:lb + boff + d * 8 + c + 1]
                    for bi, (b0, bw) in enumerate(blocks):
                        bank, bb = next_bank()
                        mm_group(bank, bb, (0, bw), [(gw[:, gi * 2 + d, :], vbf[:, b0:b0 + bw])], [gw_b, vbf_b])
                        P.op("act", lambda e, o=dst[:, b0:b0 + bw], i=bank[:, 0:bw], bias=bias: e.activation(out=o, in_=i, func=AF.Sigmoid, bias=bias),
                             reads=[bb, par_b], pwrites=[dst_b])
                sc1 = s8[:, l * 32 + d * 8 + c:l * 32 + d * 8 + c + 1]
                sc2 = s8[:, l * 32 + 16 + d * 8 + c:l * 32 + 16 + d * 8 + c + 1]
                act_op(a_[:], r_[:], AF.Exp, [r_b, s8_bufs[l]], [a_b], scale=sc1)
                act_op(m_[:], r_[:], AF.Exp, [r_b, s8_bufs[l]], [m_b], scale=sc2)
                act_op(m_[:], m_[:], AF.Sqrt, [m_b, misc_b], [m_b], bias=oneb[:], scale=-1.0)
                dve(lambda e: e.tensor_tensor(out=i_[:], in0=i_[:], in1=v[:], op=ALU.mult), [i_b, v_b], [i_b])
                dve(lambda e: e.tensor_tensor(out=i_[:], in0=i_[:], in1=m_[:], op=ALU.mult), [i_b, m_b], [i_b])
                if d == 0:
                    dve(lambda e: e.tensor_tensor_scan(out=hf[:], data0=a_[:], data1=i_[:], initial=0.0, op0=ALU.mult, op1=ALU.add),
                        [a_b, i_b], [hf_b])
                else:
                    dve(lambda e: e.tensor_tensor_scan(out=r_[:, 0:CTX][:, ::-1], data0=a_[:, 0:CTX][:, ::-1], data1=i_[:, 0:CTX][:, ::-1],
                                                       initial=0.0, op0=ALU.mult, op1=ALU.add), [a_b, i_b, r_b], [r_b])
                    dve(lambda e: e.tensor_tensor_scan(out=r_[:, CTX:N][:, ::-1], data0=a_[:, CTX:N][:, ::-1], data1=i_[:, CTX:N][:, ::-1],
                                                       initial=r_[:, 0:1], op0=ALU.mult, op1=ALU.add), [a_b, i_b, r_b], [r_b])
            dve(lambda e: e.tensor_tensor(out=hf[:], in0=hf[:], in1=r_[:], op=ALU.add), [hf_b, r_b], [hf_b])
            dve(lambda e: e.tensor_tensor(out=zst[:], in0=hf[:], in1=gg[:], op=ALU.mult), [hf_b, gg_b], [zst_b])
            dma("sp", zTd[c], zst[:], zT_b, reads=[zst_b], pwrites=[zT_b])
            if hook is not None:
                hook()

    def phaseF(l, S):
        cv.reset()
        L = CTX if S == "ctx" else SEQ
        ntc = L // 128
        Psb = cv.take(ntc * 2048, BF16).rearrange("p (a c j) -> p a c j", a=ntc, c=2)
        Psb_b = pb("Psb")
        yst = [cv.take(8 * TT, BF16).rearrange("p (k t) -> p k t", k=8) for _ in range(2)]
        yst_b = [[Buf("yst%d_%d" % (i, k)) for k in range(8)] for i in range(2)]
        tp = tpc_d if S == "ctx" else tpl_d
        for a0 in range(0, ntc, 4):
            na = min(4, ntc - a0)
            dma("sp", Psb[:, a0:a0 + na], Pd[S][a0 * 128:(a0 + na) * 128].rearrange("(a p) c j -> p a c j", p=128), Psb_b,
                reads=[Pd_b[S]], pwrites=[Psb_b])
        for ti, (t0, T) in enumerate(tile_list(S)):
            slots = []
            for cs in range(2):
                slot, slb = next_slot()
                src = tp[cs, :, t0:t0 + T].rearrange("(a p) k -> p a k", p=128)
                dma("pool", slot[:, 0:ntc, 0:T], src, slb, writes=[slb], nobar=True)
                slots.append((slot, slb))
            si = ti % 2
            for jc in range(8):
                bank, bb = next_bank()
                pairs = []
                for cs in range(2):
                    for a in range(ntc):
                        pairs.append((Psb[:, a, cs, jc * 128:(jc + 1) * 128], slots[cs][0][:, a, 0:T]))
                mm_group(bank, bb, (0, T), pairs, [Psb_b, slots[0][1], slots[1][1]])
                copy_op(ev_eng(), yst[si][:, jc, 0:T], bank[:, 0:T], [bb], [yst_b[si][jc]])
            c0 = COL0[S] + t0
            dma("sp", YTd.rearrange("k p t -> p k t")[:, :, c0:c0 + T], yst[si][:, :, 0:T], YT_b[S][ti],
                reads=yst_b[si], writes=[YT_b[S][ti]])

    def phaseC(l, S, ti, t0, T):
        cv.reset()
        c0 = COL0[S] + t0
        yt = cv.take(8 * TT, BF16).rearrange("p (k t) -> p k t", k=8)
        zt = cv.take(8 * TT, BF16).rearrange("p (k t) -> p k t", k=8)
        sgf = [cv.take(4 * TT, F32).rearrange("p (j t) -> p j t", j=4) for _ in range(2)]
        sgr = [cv.take(4 * TT, F32).rearrange("p (j t) -> p j t", j=4) for _ in range(2)]
        t1 = [cv.take(TT, F32) for _ in range(2)]
        t2 = [cv.take(TT, F32) for _ in range(2)]
        mg = cv.take(KC * TT, BF16).rearrange("p (k t) -> p k t", k=KC)
        yt_b, zt_b = pb("yt"), pb("zt")
        sgf_b = [pb("sgf0"), pb("sgf1")]
        sgr_b = [pb("sgr0"), pb("sgr1")]
        t1_b = [Buf("t1_0"), Buf("t1_1")]
        t2_b = [Buf("t2_0"), Buf("t2_1")]
        mg_b = [Buf("mg%d" % k) for k in range(KC)]
        dma("sp", res[:, :, 0:T], resT[S].rearrange("k p t -> p k t")[:, :, t0:t0 + T], res_b[0],
            reads=[resT_b[S][ti]], writes=res_b, nobar=True)
        dma("sp", yt[:, :, 0:T], YTd.rearrange("k p t -> p k t")[:, :, c0:c0 + T], yt_b, reads=[YT_b[S][ti]], writes=[yt_b])
        dma("sp", zt[:, :, 0:T], zTd.rearrange("k p t -> p k t")[:, :, c0:c0 + T], zt_b, reads=[zT_b], writes=[zt_b])
        for pn in range(4):
            slot, slb = next_slot()
            load_w(w_fo[l], 0, 8, pn * 512, 512, slot, slb, kc_off=0, first=True)
            load_w(w_ro[l], 0, 8, pn * 512, 512, slot, slb, kc_off=8, first=False)
            si = pn % 2
            dma("sp", sgf[si][:, :, 0:T], sgfT[pn * 4:pn * 4 + 4].rearrange("k p t -> p k t")[:, :, c0:c0 + T], sgf_b[si],
                reads=[proj_b[S][ti]], writes=[sgf_b[si]])
            dma("sp", sgr[si][:, :, 0:T], sgrT[pn * 4:pn * 4 + 4].rearrange("k p t -> p k t")[:, :, c0:c0 + T], sgr_b[si],
                reads=[proj_b[S][ti]], writes=[sgr_b[si]])
            for j in range(4):
                n = pn * 4 + j
                i2 = n % 2
                bF, bFb = next_bank()
                bR, bRb = next_bank()
                mm_group(bF, bFb, (0, T), [(slot[:, k, j * 128:(j + 1) * 128], yt[:, k, 0:T]) for k in range(8)], [slb, yt_b])
                mm_group(bR, bRb, (0, T), [(slot[:, 8 + k, j * 128:(j + 1) * 128], zt[:, k, 0:T]) for k in range(8)], [slb, zt_b])
                dve(lambda e, o=t1[i2], b=bF, s=sgf[si], j=j: e.tensor_tensor(out=o[:, 0:T], in0=b[:, 0:T], in1=s[:, j, 0:T], op=ALU.mult),
                    [bFb, sgf_b[si]], [t1_b[i2]])
                dve(lambda e, o=t2[i2], b=bR, s=sgr[si], j=j: e.tensor_tensor(out=o[:, 0:T], in0=b[:, 0:T], in1=s[:, j, 0:T], op=ALU.mult),
                    [bRb, sgr_b[si]], [t2_b[i2]])
                dve(lambda e, n=n, i2=i2: e.tensor_tensor(out=mg[:, n, 0:T], in0=t1[i2][:, 0:T], in1=t2[i2][:, 0:T], op=ALU.add),
                    [t1_b[i2], t2_b[i2]], [mg_b[n]])
        g1 = modv(l, S, 2)
        for pn in range(4):
            slot, slb = next_slot()
            load_w(w_o[l], 0, KC, pn * 512, 512, slot, slb)
            for j in range(4):
                dch = pn * 4 + j
                bank, bb = next_bank()
                mm_group(bank, bb, (0, T), [(slot[:, k, j * 128:(j + 1) * 128], mg[:, k, 0:T]) for k in range(KC)], [slb] + mg_b)
                dve(lambda e, dch=dch, bank=bank: e.scalar_tensor_tensor(out=res[:, dch, 0:T], in0=bank[:, 0:T], scalar=g1[:, dch:dch + 1],
                                                                          in1=res[:, dch, 0:T], op0=ALU.mult, op1=ALU.add),
                    [bb, mod_b, res_b[dch]], [res_b[dch]])

    def swiglu_expert(T, h2, h2_b, Wg, Wu, Wd, nf, actT, actT_b, epilogue):
        sg = [cv_sg[0], cv_sg[1]]
        npan = nf // 4
        for fp in range(npan):
            sG, sGb = next_slot()
            load_w(Wg, 0, KC, fp * 512, 512, sG, sGb)
            sU, sUb = next_slot()
            load_w(Wu, 0, KC, fp * 512, 512, sU, sUb)
            for j in range(4):
                f = fp * 4 + j
                i2 = f % 2
                bG, bGb = next_bank()
                bU, bUb = next_bank()
                mm_group(bG, bGb, (0, T), [(sG[:, k, j * 128:(j + 1) * 128], h2[:, k, 0:T]) for k in range(KC)], [sGb] + h2_b)
                mm_group(bU, bUb, (0, T), [(sU[:, k, j * 128:(j + 1) * 128], h2[:, k, 0:T]) for k in range(KC)], [sUb] + h2_b)
                act_op(sg[i2][:, 0:T], bG[:, 0:T], AF.Silu, [bGb], [cv_sg_b[i2]])
                dve(lambda e, f=f, i2=i2, bU=bU: e.tensor_tensor(out=actT[:, f, 0:T], in0=bU[:, 0:T], in1=sg[i2][:, 0:T], op=ALU.mult),
                    [bUb, cv_sg_b[i2]], [actT_b[f]])
        slabs = []
        k0 = 0
        while k0 < nf:
            nk = min(14 if nf % 14 == 0 else 16, nf - k0)
            slabs.append((k0, nk))
            k0 += nk
        for dp in range(4):
            banks = [next_bank() for _ in range(4)]
            for si, (k0, nk) in enumerate(slabs):
                slot, slb = next_slot()
                load_w(Wd, k0, nk, dp * 512, 512, slot, slb)
                for j in range(4):
                    bank, bb = banks[j]
                    mm_group(bank, bb, (0, T), [(slot[:, k, j * 128:(j + 1) * 128], actT[:, k0 + k, 0:T]) for k in range(nk)],
                             [slb] + actT_b[k0:k0 + nk], first=(si == 0), last=(si == len(slabs) - 1))
            for j in range(4):
                epilogue(dp * 4 + j, banks[j][0], banks[j][1])

    cv_sg = [None, None]
    cv_sg_b = [Buf("sg0"), Buf("sg1")]

    def phaseD(l, S, ti, t0, T):
        cv.reset()
        last = (l == DEPTH - 1)
        moe = (l % 2 == 1)
        h2 = cv.take(KC * TT, BF16).rearrange("p (k t) -> p k t", k=KC)
        h2_b = [Buf("h2_%d" % k) for k in range(KC)]
        scr = norm_scratch()
        cv_sg[0] = cv.take(TT, F32)
        cv_sg[1] = cv.take(TT, F32)
        nf = (D_EXP if moe else D_FF) // 128
        actT = cv.take(nf * TT, BF16).rearrange("p (k t) -> p k t", k=nf)
        actT_b = [Buf("act%d" % k) for k in range(nf)]
        g2 = modv(l, S, 5)
        if not moe:
            norm_mod(T, drvA(l, S, 1), modv(l, S, 3), h2, h2_b, scr)

            def epi(dch, bank, bb):
                dve(lambda e, dch=dch, bank=bank: e.scalar_tensor_tensor(out=res[:, dch, 0:T], in0=bank[:, 0:T], scalar=g2[:, dch:dch + 1],
                                                                          in1=res[:, dch, 0:T], op0=ALU.mult, op1=ALU.add),
                    [bb, mod_b, res_b[dch]], [res_b[dch]])
            swiglu_expert(T, h2, h2_b, ffn_g[0], ffn_u[0], ffn_d[0], nf, actT, actT_b, epi)
        else:
            ntb = T // 128
            Bsb = cv.take(2 * TT, F32).rearrange("p (e t) -> p e t", e=2)
            Bsb_b = [Buf("Bsb0"), Buf("Bsb1")]
            h2f = scr[0]
            h2f_b = scr[1]
            diag = [cv.take(128, F32) for _ in range(2)]
            diag_b = [Buf("diag0"), Buf("diag1")]
            sm = cv.take(64, F32)
            sm_b = Buf("sm")
            comb = cv.take(ntb * NEXP, F32).rearrange("p (a e) -> p a e", a=ntb)
            comb_b = [Buf("comb%d" % a) for a in range(ntb)]
            tmp = scr[2]
            tmp_b = scr[3]
            lbanks = [next_bank() for _ in range(ntb)]
            Bv = modv(l, S, 3)

            def extra(k, xs_ap, xs_buf):
                i2 = k % 2
                act_op(h2f[i2][:, 0:T], xs_ap[:, 0:T], AF.Identity, [xs_buf, mod_b], [h2f_b[i2]], bias=Bv[:, k:k + 1])
                copy_op("dve", h2[:, k, 0:T], h2f[i2][:, 0:T], [h2f_b[i2]], [h2_b[k]])
                for a in range(ntb):
                    bank, bb = lbanks[a]
                    mm_group(bank, bb, (0, NEXP), [(h2f[i2][:, a * 128:(a + 1) * 128], router_sb[:, k, :])], [h2f_b[i2], rt_b],
                             first=(k == 0), last=(k == KC - 1))
            norm_mod(T, drvA(l, S, 1), None, h2, h2_b, scr, extra=extra)
            for a in range(ntb):
                bank, bb = lbanks[a]
                lg = sm[:, 0:8]
                eq1 = sm[:, 8:16]
                lg2 = sm[:, 16:24]
                eq2 = sm[:, 24:32]
                m1 = sm[:, 32:33]
                m2 = sm[:, 33:34]
                ee = sm[:, 34:35]
                g1_ = sm[:, 35:36]
                g2_ = sm[:, 36:37]
                c1 = sm[:, 40:48]
                R = [sm_b]
                dve(lambda e, bank=bank: e.tensor_copy(out=lg, in_=bank[:, 0:8]), [bb, sm_b], R)
                dve(lambda e: e.reduce_max(out=m1, in_=lg, axis=AX.X), R, R)
                dve(lambda e: e.tensor_scalar(out=eq1, in0=lg, scalar1=m1, scalar2=None, op0=ALU.is_equal), R, R)
                dve(lambda e: e.scalar_tensor_tensor(out=lg2, in0=eq1, scalar=-1e30, in1=lg, op0=ALU.mult, op1=ALU.add), R, R)
                dve(lambda e: e.reduce_max(out=m2, in_=lg2, axis=AX.X), R, R)
                dve(lambda e: e.tensor_scalar(out=eq2, in0=lg2, scalar1=m2, scalar2=None, op0=ALU.is_equal), R, R)
                dve(lambda e: e.tensor_tensor(out=ee, in0=m2, in1=m1, op=ALU.subtract), R, R)
                act_op(ee, ee, AF.Exp, R, R)
                dve(lambda e: e.tensor_scalar(out=g1_, in0=ee, scalar1=1.0, scalar2=None, op0=ALU.add), R, R)
                dve(lambda e: e.reciprocal(out=g1_, in_=g1_), R, R)
                dve(lambda e: e.tensor_tensor(out=g2_, in0=ee, in1=g1_, op=ALU.mult), R, R)
                dve(lambda e: e.tensor_scalar(out=c1, in0=eq1, scalar1=g1_, scalar2=None, op0=ALU.mult), R, R)
                dve(lambda e, a=a: e.scalar_tensor_tensor(out=comb[:, a, :], in0=eq2, scalar=g2_, in1=c1, op0=ALU.mult, op1=ALU.add),
                    R, [comb_b[a]])
            dctr = 0
            ectr2 = [0]
            for ex in range(NEXP):
                bank, bb = next_bank()
                bx = ex % 2
                for a in range(ntb):
                    i2 = dctr % 2
                    dctr += 1
                    dve(lambda e, i2=i2, a=a, ex=ex: e.tensor_scalar(out=diag[i2][:], in0=ident[:], scalar1=comb[:, a, ex:ex + 1], scalar2=None, op0=ALU.mult),
                        [comb_b[a], consts_b], [diag_b[i2]])
                    mm_group(bank, bb, (a * 128, (a + 1) * 128), [(ones[:], diag[i2][:])], [diag_b[i2], misc_b], fresh=(a == 0))
                copy_op("act", Bsb[:, bx, 0:T], bank[:, 0:T], [bb], [Bsb_b[bx]])

                def epi(dch, bank, bb, bx=bx):
                    i2 = ectr2[0] % 2
                    ectr2[0] += 1
                    dve(lambda e, dch=dch, bank=bank, i2=i2, bx=bx: e.scalar_tensor_tensor(out=tmp[i2][:, 0:T], in0=bank[:, 0:T], scalar=g2[:, dch:dch + 1],
                                                                                            in1=Bsb[:, bx, 0:T], op0=ALU.mult, op1=ALU.mult),
                        [bb, mod_b, Bsb_b[bx]], [tmp_b[i2]])
                    dve(lambda e, dch=dch, i2=i2: e.tensor_tensor(out=res[:, dch, 0:T], in0=res[:, dch, 0:T], in1=tmp[i2][:, 0:T], op=ALU.add),
                        [tmp_b[i2], res_b[dch]], [res_b[dch]])
                swiglu_expert(T, h2, h2_b, moe_g[0, ex], moe_u[0, ex], moe_d[0, ex], nf, actT, actT_b, epi)
        if not last:
            dma("sp", resT[S].rearrange("k p t -> p k t")[:, :, t0:t0 + T], res[:, :, 0:T], resT_b[S][ti],
                reads=res_b, writes=[resT_b[S][ti]])
        elif S == "lat":
            P.barrier()
            cv.reset()
            scr = norm_scratch()
            sq, sq_b, xs, xs_b, rstd, rstd_b = scr
            yf = cv.take(KC * TT, F32).rearrange("p (k t) -> p k t", k=KC)
            yf_b = [Buf("yf%d" % k) for k in range(KC)]
            otok = [cv.take(D, F32) for _ in range(2)]
            otok_b = [[Buf("otok%d_%d" % (i, q)) for q in range(4)] for i in range(2)]
            bank, bb = next_bank()
            for k in range(KC):
                i2 = k % 2
                act_op(sq[i2][:, 0:T], res[:, k, 0:T], AF.Square, [res_b[k]], [sq_b[i2]])
                mm_group(bank, bb, (0, T), [(ones[:], sq[i2][:, 0:T])], [sq_b[i2], misc_b], first=(k == 0), last=(k == KC - 1))
            act_op(rstd[:, 0:T], bank[:, 0:T], AF.Sqrt, [bb, misc_b], [rstd_b], bias=epsb[:], scale=1.0 / D)
            dve(lambda e: e.reciprocal(out=rstd[:, 0:T], in_=rstd[:, 0:T]), [rstd_b], [rstd_b])
            gfin = par[:, P_FIN:P_FIN + 16]
            for k in range(KC):
                dve(lambda e, k=k: e.scalar_tensor_tensor(out=yf[:, k, 0:T], in0=res[:, k, 0:T], scalar=gfin[:, k:k + 1], in1=rstd[:, 0:T],
                                                          op0=ALU.mult, op1=ALU.mult), [res_b[k], rstd_b, par_b], [yf_b[k]])
            for a in range(T // 128):
                oi = a % 2
                for q in range(4):
                    bank, bb = next_bank()

                    def fn(e, bank=bank, a=a, q=q):
                        ins = None
                        for kk in range(4):
                            k = q * 4 + kk
                            ins = e.transpose(bank[:, kk * 128:(kk + 1) * 128], yf[:, k, a * 128:(a + 1) * 128], ident[:])
                        return ins
                    P.op("pe", fn, reads=yf_b[q * 4:q * 4 + 4] + [consts_b], writes=[bb])
                    copy_op(ev_eng(), otok[oi][:, q * 512:(q + 1) * 512], bank[:, 0:512], [bb], [otok_b[oi][q]])
                dma("sp", out_d[t0 + a * 128:t0 + (a + 1) * 128, :], otok[oi][:], out_b, reads=otok_b[oi], pwrites=[out_b])

    stages = []

    def stage(name):
        stages.append(name)
        return stop_after is not None and len(stages) > stop_after

    def run():
        ada0 = ada_steps(0)
        phase0(hook=lambda: run_steps(ada0, 6))
        run_steps(ada0, 100)
        P.barrier()
        if stage("p0ada"):
            return
        for l in range(DEPTH):
            last = (l == DEPTH - 1)
            phaseA_all(l)
            P.barrier()
            if stage("A%d" % l):
                return
            if not last:
                adan = ada_steps(l + 1)
                phaseB(l, hook=lambda: run_steps(adan, 4))
                run_steps(adan, 100)
            else:
                phaseB(l)
            P.barrier()
            if dump and last:
                dma("sp", mod_dbg, mod[:], pb("moddbg"), reads=[mod_b])
            if stage("B%d" % l):
                return
            for S in ("ctx", "lat"):
                if last and S == "ctx":
                    continue
                phaseF(l, S)
                P.barrier()
            if stage("F%d" % l):
                return
            for S in ("ctx", "lat"):
                if last and S == "ctx":
                    continue
                for ti, (t0, T) in enumerate(tile_list(S)):
                    phaseC(l, S, ti, t0, T)
                    P.barrier()
                    phaseD(l, S, ti, t0, T)
                    P.barrier()
            if stage("CD%d" % l):
                return

    run()
    fin_reads = [out_b, zT_b] + [b for S in ("ctx", "lat") for b in resT_b[S] + proj_b[S] + YT_b[S]] + [Pd_b["ctx"], Pd_b["lat"]]
    P.op("sp", None, reads=fin_reads, barrier=True)

    all_bufs_with_dma = set()
    for e in P.ENGS:
        for o in P.q[e]:
            if o.dma:
                all_bufs_with_dma.add(o.dst)
    for i, b in enumerate(sorted(all_bufs_with_dma, key=lambda b: b.name)):
        b.dsem = es.enter_context(nc.semaphore("d%d_%s" % (i, b.name)))
    sems = {e: es.enter_context(nc.semaphore("eng_" + e)) for e in P.ENGS}
    with nc.Block() as block:
        @block.tensor
        def _(e):
            P.emit_one("pe", e, sems)

        @block.scalar
        def _(e):
            P.emit_one("act", e, sems)

        @block.vector
        def _(e):
            P.emit_one("dve", e, sems)

        @block.gpsimd
        def _(e):
            P.emit_one("pool", e, sems)

        @block.sync
        def _(e):
            P.emit_one("sp", e, sems)
    es.close()
    return nc, stages


def _prog_mark(self):
    for e in self.ENGS:
        for o in self.q[e]:
            for d in o.raw + o.war:
                if not d.dma:
                    d.sig = True
    for e in self.ENGS:
        n = 0
        for o in self.q[e]:
            if o.sig and not o.dma:
                n += 1
                o.idx = n
    self.marked = True


def _prog_emit_one(self, e, eng, sems):
    if not getattr(self, "marked", False):
        _prog_mark(self)
    waited = {}

    def need(sem, val):
        if waited.get(sem.name, 0) < val:
            eng.wait_ge(sem, val)
            waited[sem.name] = val

    for o in self.q[e]:
        for lst, is_war in ((o.raw, False), (o.war, True)):
            for d in lst:
                if d.dma:
                    need(d.dst.dsem, d.dval)
                else:
                    if d.eng == e and (e in ("pe", "sp", "pool") or is_war):
                        continue
                    need(sems[d.eng], d.idx)
        ins = o.fn(eng) if o.fn is not None else None
        if o.dma:
            ins.then_inc(o.dst.dsem, 16)
        elif o.sig:
            if ins is None:
                ins = eng.nop()
            ins.then_inc(sems[e], 1)


Prog.emit_one = _prog_emit_one


def _host_consts():
    j = np.arange(256)
    ang = 2.0 * np.pi * ((j[:, None] * j[None, :]) % 256) / 256.0
    cs256 = np.concatenate([np.cos(ang), np.sin(ang)], axis=1) / 16.0

    def tp(L):
        t = np.arange(L)
        a = 2.0 * np.pi * ((t[:, None] * t[None, :]) % L) / float(L)
        return np.stack([np.cos(a), -np.sin(a)], axis=0) / np.sqrt(float(L))
    return (np.ascontiguousarray(cs256, dtype=np.float32), np.ascontiguousarray(tp(SEQ), dtype=np.float32),
            np.ascontiguousarray(tp(CTX), dtype=np.float32), np.eye(128, dtype=np.float32))


def _fm(vec, nchunk):
    return np.ascontiguousarray(np.asarray(vec, dtype=np.float32).reshape(nchunk, 128).T)


def _params(inp):
    par = np.zeros((128, NPAR), np.float32)
    for l in range(DEPTH):
        b = l * P_LSZ
        par[:, b + P_ADAB:b + P_ADAB + 96] = _fm(inp["ada_b"][l], 96)
        par[:, b + P_N1:b + P_N1 + 16] = _fm(inp["norm1_g"][l], 16)
        par[:, b + P_N2:b + P_N2 + 16] = _fm(inp["norm2_g"][l], 16)
        for k in range(4):
            par[:, b + P_CW + k * 8:b + P_CW + k * 8 + 8] = _fm(inp["conv_w"][l, k], 8)
        par[:, b + P_CB:b + P_CB + 8] = _fm(inp["conv_b"][l], 8)
        for d in range(2):
            par[:, b + P_BA + d * 8:b + P_BA + d * 8 + 8] = _fm(inp["lru_ba"][l, d], 8)
            par[:, b + P_BX + d * 8:b + P_BX + d * 8 + 8] = _fm(inp["lru_bx"][l, d], 8)
            par[:, b + P_LAM + d * 8:b + P_LAM + d * 8 + 8] = _fm(inp["lru_lambda"][l, d], 8)
    par[:, P_FIN:P_FIN + 16] = _fm(inp["final_norm_g"], 16)
    return par


_CACHE = {}


def make_in_maps(inp):
    cs256, tpl, tpc, ident = _host_consts()
    par = _params(inp)
    shared = {"params": par, "ident": ident, "cs256": cs256, "tp_lat": tpl, "tp_ctx": tpc}
    for name in ("ada_w", "w_in", "lru_wa", "lru_wx", "w_fourier_out", "w_lru_out", "w_out", "ffn_w_gate", "ffn_w_up",
                 "ffn_w_down", "moe_router", "moe_w_gate", "moe_w_up", "moe_w_down"):
        shared[name] = np.ascontiguousarray(np.asarray(inp[name], dtype=np.float32))
    maps = []
    for b in range(8):
        m = dict(shared)
        m["x"] = np.ascontiguousarray(np.asarray(inp["x"][b], dtype=np.float32))
        m["ctx"] = np.ascontiguousarray(np.asarray(inp["ctx"][b], dtype=np.float32))
        cc = np.stack([_fm(inp["c"][b], KC), _fm(inp["c_ctx"], KC)], axis=-1)
        m["cc"] = np.ascontiguousarray(cc, dtype=np.float32)
        maps.append(m)
    return maps


def kernel(**inputs):
    if "nc" not in _CACHE:
        _CACHE["nc"] = build_program()[0]
    nc = _CACHE["nc"]
    in_maps = make_in_maps(inputs)
    r = run_bass_kernel_spmd(nc, in_maps, core_ids=list(range(8)))
    return np.stack([np.asarray(r.results[b]["out"], dtype=np.float32) for b in range(8)], axis=0)
```

```python
import numpy as np
import concourse.bass as bass
import concourse.mybir as mybir
from concourse.bass_utils import run_bass_kernel_spmd

F32 = mybir.dt.float32
BF16 = mybir.dt.bfloat16
U8 = mybir.dt.uint8
AF = mybir.ActivationFunctionType
ALU = mybir.AluOpType
AX = mybir.AxisListType

D = 2048
KC = 16
SEQ = 2048
CTX = 256
NTOK = CTX + SEQ
DEPTH = 2
N_IN = 7168
D_FF = 5632
D_EXP = 7168
NEXP = 8
EPS = 1e-6
TT = 512

P_ADAB = 0
P_N1 = 96
P_N2 = 112
P_CW = 128
P_CB = 160
P_BA = 168
P_BX = 184
P_LAM = 200
P_LSZ = 216
P_FIN = DEPTH * P_LSZ
NPAR = P_FIN + 16

DEBUG = {"stop_after": None, "dump": False}


class Buf:
    __slots__ = ("name", "writers", "readers", "prev_readers", "dsem", "dcount")

    def __init__(self, name):
        self.name = name
        self.writers = []
        self.readers = []
        self.prev_readers = []
        self.dsem = None
        self.dcount = 0


class Op:
    __slots__ = ("eng", "fn", "raw", "war", "dma", "dst", "dval", "sig", "idx")

    def __init__(self, eng, fn):
        self.eng = eng
        self.fn = fn
        self.raw = []
        self.war = []
        self.dma = False
        self.dst = None
        self.dval = 0
        self.sig = False
        self.idx = 0


class Prog:
    ENGS = ("pe", "act", "dve", "pool", "sp")

    def __init__(self, nc):
        self.nc = nc
        self.q = {e: [] for e in self.ENGS}
        self.bar = Buf("BAR")
        self.nops = 0

    @staticmethod
    def _add_reader(lst, o):
        if not o.dma:
            for i, r in enumerate(lst):
                if (not r.dma) and r.eng == o.eng:
                    lst[i] = o
                    return
        lst.append(o)

    def op(self, eng, fn, reads=(), writes=(), pwrites=(), dma_dst=None, barrier=False, nobar=False):
        o = Op(eng, fn)
        if dma_dst is not None:
            o.dma = True
            o.dst = dma_dst
            dma_dst.dcount += 1
            o.dval = 16 * dma_dst.dcount
        rd = list(reads)
        wr = list(writes)
        if barrier:
            wr.append(self.bar)
        elif not nobar:
            rd.append(self.bar)
        for b in rd:
            o.raw.extend(b.writers)
            self._add_reader(b.readers, o)
        for b in wr:
            o.raw.extend(b.writers)
            if b is self.bar:
                o.raw.extend(b.readers)
            else:
                o.war.extend(b.readers)
            o.war.extend(b.prev_readers)
            b.writers = [o]
            b.readers = []
            b.prev_readers = []
        for b in pwrites:
            o.war.extend(b.readers)
            o.war.extend(b.prev_readers)
            if b.readers:
                b.prev_readers = b.readers
                b.readers = []
                b.writers = []
            self._add_reader(b.writers, o)
        self.q[eng].append(o)
        self.nops += 1
        return o

    def barrier(self):
        self.op("dve", None, barrier=True)

    def emit(self, block_engines, sems):
        for e in self.ENGS:
            for o in self.q[e]:
                for d in o.raw + o.war:
                    if not d.dma:
                        d.sig = True
        for e in self.ENGS:
            n = 0
            for o in self.q[e]:
                if o.sig and not o.dma:
                    n += 1
                    o.idx = n
        for e in self.ENGS:
            eng = block_engines[e]
            waited = {}
            pending_inc = [0]

            def need(sem, val, waited=waited, eng=eng):
                if waited.get(sem.name, 0) < val:
                    eng.wait_ge(sem, val)
                    waited[sem.name] = val

            for o in self.q[e]:
                for d, is_war in [(x, False) for x in o.raw] + [(x, True) for x in o.war]:
                    if d.dma:
                        need(d.dst.dsem, d.dval)
                    else:
                        if d.eng == e:
                            if e in ("pe", "sp", "pool") or is_war:
                                continue
                        need(sems[d.eng], d.idx)
                ins = o.fn(eng) if o.fn is not None else None
                if o.dma:
                    ins.then_inc(o.dst.dsem, 16)
                elif o.sig:
                    if ins is None:
                        ins = eng.nop()
                    ins.then_inc(sems[e], 1)


def build_program(stop_after=None, dump=False):
    nc = bass.Bass("TRN2", target_bir_lowering=False)
    from contextlib import ExitStack
    es = ExitStack()

    def din(name, shape, dt=F32):
        return nc.dram_tensor(name, list(shape), dt, kind="ExternalInput").ap()

    def dscr(name, shape, dt=F32):
        return nc.dram_tensor(name, list(shape), dt, kind=("ExternalOutput" if dump else "Internal")).ap()

    x_d = din("x", [SEQ, D])
    ctx_d = din("ctx", [CTX, D])
    cc_d = din("cc", [128, KC, 2])
    par_d = din("params", [128, NPAR])
    ident_d = din("ident", [128, 128])
    cs_d = din("cs256", [256, 512])
    tpl_d = din("tp_lat", [2, SEQ, SEQ])
    tpc_d = din("tp_ctx", [2, CTX, CTX])
    ada_w = din("ada_w", [DEPTH, D, 6 * D])
    w_in = din("w_in", [DEPTH, D, N_IN])
    lru_wa = din("lru_wa", [DEPTH, 2, 8, 128, 128])
    lru_wx = din("lru_wx", [DEPTH, 2, 8, 128, 128])
    w_fo = din("w_fourier_out", [DEPTH, 1024, D])
    w_ro = din("w_lru_out", [DEPTH, 1024, D])
    w_o = din("w_out", [DEPTH, D, D])
    ffn_g = din("ffn_w_gate", [1, D, D_FF])
    ffn_u = din("ffn_w_up", [1, D, D_FF])
    ffn_d = din("ffn_w_down", [1, D_FF, D])
    router_d = din("moe_router", [1, D, NEXP])
    moe_g = din("moe_w_gate", [1, NEXP, D, D_EXP])
    moe_u = din("moe_w_up", [1, NEXP, D, D_EXP])
    moe_d = din("moe_w_down", [1, NEXP, D_EXP, D])
    out_d = nc.dram_tensor("out", [SEQ, D], F32, kind="ExternalOutput").ap()

    resT = {"lat": dscr("resT_lat", [KC, 128, SEQ]), "ctx": dscr("resT_ctx", [KC, 128, CTX])}
    urT = dscr("urT", [8, 128, NTOK])
    ggT = dscr("ggT", [8, 128, NTOK])
    sgfT = dscr("sgfT", [KC, 128, NTOK])
    sgrT = dscr("sgrT", [KC, 128, NTOK])
    Pd = {"lat": dscr("P_lat", [SEQ, 2, 1024], BF16), "ctx": dscr("P_ctx", [CTX, 2, 1024], BF16)}
    YTd = dscr("YT", [8, 128, NTOK], BF16)
    zTd = dscr("zT", [8, 128, NTOK], BF16)
    mod_dbg = dscr("mod_dbg", [128, DEPTH * 2 * 96]) if dump else None

    P = Prog(nc)

    def sb(name, shape, dt):
        return es.enter_context(nc.sbuf_tensor("s_" + name, list(shape), dt))

    WSLOTS = 4
    wslot = [sb("wslot%d" % i, [128, 16, 512], BF16) for i in range(WSLOTS)]
    wslot_b = [Buf("wslot%d" % i) for i in range(WSLOTS)]
    wctr = [0]

    def next_slot():
        i = wctr[0] % WSLOTS
        wctr[0] += 1
        return wslot[i], wslot_b[i]

    res = sb("res", [128, KC, TT], F32)
    res_b = [Buf("res%d" % k) for k in range(KC)]
    ident = sb("ident", [128, 128], F32)
    ones = sb("ones", [128, 128], F32)
    cs_sb = sb("cs_sb", [128, 2, 512], BF16)
    par = sb("par", [128, NPAR], F32)
    mod = sb("mod", [128, DEPTH * 2 * 96], F32)
    drv = sb("drv", [128, DEPTH * 2 * 64], F32)
    s8 = sb("s8", [128, DEPTH * 32], F32)
    cc_sb = sb("cc_sb", [128, KC, 2], F32)
    sc_bf = sb("sc_bf", [128, KC, 2], BF16)
    router_sb = sb("router_sb", [128, KC, NEXP], F32)
    epsb = sb("epsb", [128, 1], F32)
    oneb = sb("oneb", [128, 1], F32)
    ARENA = 100 * 1024
    arena = sb("arena", [128, ARENA], U8)
    consts_b = Buf("consts")
    par_b = Buf("par")
    mod_b = Buf("mod")
    drv_b = Buf("drv")

    class Carve:
        def __init__(self):
            self.off = 0

        def reset(self):
            self.off = 0

        def take(self, nelem, dt, shape=None):
            esz = 4 if dt == F32 else 2
            nb = nelem * esz
            nb = (nb + 63) // 64 * 64
            assert self.off + nb <= ARENA, ("arena overflow", self.off, nb)
            a = arena[:, self.off:self.off + nelem * esz].bitcast(dt)
            self.off += nb
            return a

    cv = Carve()
    _pb = {}

    def pb(name):
        if name not in _pb:
            _pb[name] = Buf(name)
        return _pb[name]

    psum = [es.enter_context(nc.psum_tensor("ps%d" % i, [128, 512], F32)) for i in range(8)]
    psum_b = [Buf("ps%d" % i) for i in range(8)]
    pctr = [0]

    def next_bank():
        i = pctr[0] % 8
        pctr[0] += 1
        return psum[i], psum_b[i]

    ectr = [0]

    def ev_eng():
        ectr[0] += 1
        return "act" if ectr[0] % 2 else "dve"

    def dma(eng, out_ap, in_ap, dst_buf, reads=(), writes=(), pwrites=(), nobar=False):
        fn = lambda e, o=out_ap, i=in_ap: e.dma_start(out=o, in_=i)
        return P.op(eng, fn, reads=reads, writes=writes, pwrites=pwrites, dma_dst=dst_buf, nobar=nobar)

    def load_w(dram_ap_2d, k0, nk, c0, ncol, slot, slot_buf, kc_off=0, first=True):
        src = dram_ap_2d[k0 * 128:(k0 + nk) * 128, c0:c0 + ncol].rearrange("(k p) n -> p k n", p=128)
        dst = slot[:, kc_off:kc_off + nk, 0:ncol]
        if first:
            return dma("pool", dst, src, slot_buf, writes=[slot_buf], nobar=True)
        return dma("pool", dst, src, slot_buf, pwrites=[slot_buf], nobar=True)

    def mm_group(bank, bank_b, cols, pairs, reads, first=True, last=True, fresh=None):
        if fresh is None:
            fresh = first
        def fn(e, bank=bank, cols=cols, pairs=pairs, first=first, last=last):
            n = len(pairs)
            ins = None
            for i, (l, r) in enumerate(pairs):
                ins = e.matmul(bank[:, cols[0]:cols[1]], l, r, start=(first and i == 0), stop=(last and i == n - 1))
            return ins
        if fresh:
            return P.op("pe", fn, reads=reads, writes=[bank_b])
        return P.op("pe", fn, reads=reads, pwrites=[bank_b])

    def act_op(out_ap, in_ap, func, reads, writes, bias=None, scale=None, eng="act"):
        kw = {}
        if bias is not None:
            kw["bias"] = bias
        if scale is not None:
            kw["scale"] = scale
        return P.op("act", lambda e, o=out_ap, i=in_ap, f=func, kw=kw: e.activation(out=o, in_=i, func=f, **kw),
                    reads=reads, writes=writes)

    def copy_op(eng, out_ap, in_ap, reads, writes):
        if eng == "act":
            return P.op("act", lambda e, o=out_ap, i=in_ap: e.activation(out=o, in_=i, func=AF.Copy), reads=reads, writes=writes)
        return P.op("dve", lambda e, o=out_ap, i=in_ap: e.tensor_copy(out=o, in_=i), reads=reads, writes=writes)

    def dve(fn, reads, writes):
        return P.op("dve", fn, reads=reads, writes=writes)

    dma("sp", ident[:], ident_d, consts_b, writes=[consts_b])
    dma("sp", par[:], par_d, par_b, writes=[par_b])
    cc_b = Buf("cc")
    dma("sp", cc_sb[:], cc_d, cc_b, writes=[cc_b])
    rt_b = Buf("router")
    dma("sp", router_sb[:], router_d[0].rearrange("(k p) e -> p k e", p=128), rt_b, writes=[rt_b])
    csb_b = Buf("cs")
    dma("pool", cs_sb[:], cs_d.rearrange("(c p) n -> p c n", p=128), csb_b, writes=[csb_b])
    misc_b = Buf("misc")
    dve(lambda e: e.memset(ones[:], 1.0), [], [misc_b])
    dve(lambda e: e.memset(epsb[:], EPS), [], [misc_b])
    dve(lambda e: e.memset(oneb[:], 1.0), [], [misc_b])

    def tile_list(S):
        if S == "ctx":
            return [(0, CTX)]
        return [(t * TT, TT) for t in range(SEQ // TT)]

    COL0 = {"ctx": 0, "lat": CTX}
    SIDX = {"lat": 0, "ctx": 1}
    resT_b = {S: [Buf("resT_%s%d" % (S, i)) for i in range(len(tile_list(S)))] for S in ("ctx", "lat")}
    proj_b = {S: [Buf("proj_%s%d" % (S, i)) for i in range(len(tile_list(S)))] for S in ("ctx", "lat")}
    Pd_b = {S: Buf("Pd_%s" % S) for S in ("ctx", "lat")}
    YT_b = {S: [Buf("YT_%s%d" % (S, i)) for i in range(len(tile_list(S)))] for S in ("ctx", "lat")}
    zT_b = Buf("zT")
    out_b = Buf("out")

    def phase0(hook=None):
        cv.reset()
        xin = cv.take(4 * D, F32).rearrange("p (a f) -> p a f", a=4)
        xin_b = pb("xin")
        for S, src in (("ctx", ctx_d), ("lat", x_d)):
            for ti, (t0, T) in enumerate(tile_list(S)):
                ntb = T // 128
                dma("sp", xin[:, 0:ntb, :], src[t0:t0 + T, :].rearrange("(a p) f -> p a f", p=128), xin_b, writes=[xin_b])
                for k in range(KC):
                    bank, bb = next_bank()

                    def fn(e, bank=bank, k=k, ntb=ntb):
                        ins = None
                        for tb in range(ntb):
                            ins = e.transpose(bank[:, tb * 128:(tb + 1) * 128], xin[:, tb, k * 128:(k + 1) * 128], ident[:])
                        return ins
                    P.op("pe", fn, reads=[xin_b, consts_b], writes=[bb])
                    copy_op(ev_eng(), res[:, k, 0:T], bank[:, 0:T], [bb], [res_b[k]])
                dma("sp", resT[S].rearrange("k p t -> p k t")[:, :, t0:t0 + T], res[:, :, 0:T], resT_b[S][ti],
                    reads=res_b, writes=[resT_b[S][ti]])
                if hook is not None:
                    hook()

    def ada_steps(l):
        sc_b = Buf("sc_bf%d" % l)
        adab = par[:, l * P_LSZ + P_ADAB:l * P_LSZ + P_ADAB + 96]
        modl = mod[:, l * 192:(l + 1) * 192].rearrange("p (s n) -> p s n", s=2)
        steps = []

        def start():
            act_op(sc_bf[:], cc_sb[:], AF.Silu, [cc_b], [sc_b])

        def panel(pn):
            slot, slb = next_slot()
            load_w(ada_w[l], 0, KC, pn * 512, 512, slot, slb)
            bank, bb = next_bank()
            for j in range(4):
                pairs = [(slot[:, k, j * 128:(j + 1) * 128], sc_bf[:, k, :]) for k in range(KC)]
                mm_group(bank, bb, (2 * j, 2 * j + 2), pairs, [slb, sc_b], fresh=(j == 0))
            for j in range(4):
                n = pn * 4 + j
                P.op("dve", lambda e, o=modl[:, :, n], i=bank[:, 2 * j:2 * j + 2], s=adab[:, n:n + 1]:
                     e.tensor_scalar(out=o, in0=i, scalar1=s, scalar2=None, op0=ALU.add),
                     reads=[bb, par_b], pwrites=[mod_b])

        def finish():
            for s_ in range(2):
                for which, (goff, scoff) in enumerate(((P_N1, 16), (P_N2, 64))):
                    o = drv[:, (l * 2 + s_) * 64 + which * 16:(l * 2 + s_) * 64 + which * 16 + 16]
                    scv = modl[:, s_, scoff:scoff + 16]
                    g = par[:, l * P_LSZ + goff:l * P_LSZ + goff + 16]
                    P.op("dve", lambda e, o=o, scv=scv, g=g: e.scalar_tensor_tensor(out=o, in0=scv, scalar=1.0, in1=g, op0=ALU.add, op1=ALU.mult),
                         reads=[mod_b, par_b], pwrites=[drv_b])
            lam = par[:, l * P_LSZ + P_LAM:l * P_LSZ + P_LAM + 16]
            t = drv[:, (l * 2) * 64 + 32:(l * 2) * 64 + 48]
            u = drv[:, (l * 2) * 64 + 48:(l * 2) * 64 + 64]
            t2 = drv[:, (l * 2 + 1) * 64 + 32:(l * 2 + 1) * 64 + 48]
            t3 = drv[:, (l * 2 + 1) * 64 + 48:(l * 2 + 1) * 64 + 64]
            tb_ = Buf("s8tmp%d" % l)
            s8_b = s8_bufs[l]
            act_op(t, lam, AF.Exp, [par_b], [tb_], scale=-1.0)
            dve(lambda e: e.tensor_scalar(out=u, in0=t, scalar1=1.0, scalar2=None, op0=ALU.add), [tb_], [tb_])
            dve(lambda e: e.tensor_scalar(out=t2, in0=u, scalar1=-1.0, scalar2=1e-30, op0=ALU.add, op1=ALU.max), [tb_], [tb_])
            dve(lambda e: e.reciprocal(out=t2, in_=t2), [tb_], [tb_])
            act_op(t3, u, AF.Ln, [tb_], [tb_])
            dve(lambda e: e.tensor_tensor(out=t3, in0=t3, in1=t, op=ALU.mult), [tb_], [tb_])
            dve(lambda e: e.scalar_tensor_tensor(out=s8[:, l * 32:l * 32 + 16], in0=t3, scalar=-8.0, in1=t2, op0=ALU.mult, op1=ALU.mult), [tb_], [s8_b])
            dve(lambda e: e.tensor_scalar(out=s8[:, l * 32 + 16:l * 32 + 32], in0=s8[:, l * 32:l * 32 + 16], scalar1=2.0, scalar2=None, op0=ALU.mult), [s8_b], [s8_b])

        steps.append(start)
        for pn in range(24):
            steps.append(lambda pn=pn: panel(pn))
        steps.append(finish)
        return steps

    def run_steps(steps, n):
        for _ in range(n):
            if steps:
                steps.pop(0)()

    s8_bufs = [Buf("s8_%d" % l) for l in range(DEPTH)]

    def modv(l, S, idx):
        base = l * 192 + SIDX[S] * 96 + idx * 16
        return mod[:, base:base + 16]

    def drvA(l, S, which):
        base = (l * 2 + SIDX[S]) * 64 + which * 16
        return drv[:, base:base + 16]

    def norm_mod(T, Avec, Bvec, hT, hT_b, scratch, extra=None):
        sq, sq_b, xs, xs_b, rstd, rstd_b = scratch
        bank, bb = next_bank()
        for k in range(KC):
            i2 = k % 2
            act_op(sq[i2][:, 0:T], res[:, k, 0:T], AF.Square, [res_b[k]], [sq_b[i2]])
            mm_group(bank, bb, (0, T), [(ones[:], sq[i2][:, 0:T])], [sq_b[i2], misc_b], first=(k == 0), last=(k == KC - 1))
        act_op(rstd[:, 0:T], bank[:, 0:T], AF.Sqrt, [bb, misc_b], [rstd_b], bias=epsb[:], scale=1.0 / D)
        dve(lambda e: e.reciprocal(out=rstd[:, 0:T], in_=rstd[:, 0:T]), [rstd_b], [rstd_b])
        for k in range(KC):
            i2 = k % 2
            dve(lambda e, k=k, i2=i2: e.scalar_tensor_tensor(out=xs[i2][:, 0:T], in0=res[:, k, 0:T], scalar=Avec[:, k:k + 1],
                                                               in1=rstd[:, 0:T], op0=ALU.mult, op1=ALU.mult),
                [res_b[k], rstd_b, drv_b, par_b], [xs_b[i2]])
            if Bvec is not None:
                act_op(hT[:, k, 0:T], xs[i2][:, 0:T], AF.Identity, [xs_b[i2], mod_b], [hT_b[k]], bias=Bvec[:, k:k + 1])
            if extra is not None:
                extra(k, xs[i2], xs_b[i2])

    def norm_scratch():
        sq = [cv.take(TT, F32) for _ in range(2)]
        xs = [cv.take(TT, F32) for _ in range(2)]
        rstd = cv.take(TT, F32)
        return (sq, [Buf("sq0"), Buf("sq1")], xs, [Buf("xs0"), Buf("xs1")], rstd, Buf("rstd"))

    def phaseA_all(l):
        cv.reset()
        last = (l == DEPTH - 1)
        hTs = [cv.take(KC * TT, BF16).rearrange("p (k t) -> p k t", k=KC) for _ in range(2)]
        hTs_b = [[Buf("hT%d_%d" % (i, k)) for k in range(KC)] for i in range(2)]
        scr = norm_scratch()
        ufT = cv.take(8 * TT, BF16).rearrange("p (k t) -> p k t", k=8)
        ufT_b = [Buf("ufT%d" % k) for k in range(8)]
        Pt = cv.take(4 * 2048, BF16).rearrange("p (a c j) -> p a c j", a=4, c=2)
        Pt_b = Buf("Pt")
        stage = [cv.take(4 * TT, F32).rearrange("p (j t) -> p j t", j=4) for _ in range(2)]
        stage_b = [[Buf("st%d_%d" % (i, j)) for j in range(4)] for i in range(2)]
        tiles = []
        for S in ("ctx", "lat"):
            for ti, (t0, T) in enumerate(tile_list(S)):
                tiles.append((S, ti, t0, T, last and S == "ctx"))

        def prep(i):
            S, ti, t0, T, only_ur = tiles[i]
            dma("sp", res[:, :, 0:T], resT[S].rearrange("k p t -> p k t")[:, :, t0:t0 + T], res_b[0],
                reads=[resT_b[S][ti]], writes=res_b, nobar=True)
            norm_mod(T, drvA(l, S, 0), modv(l, S, 0), hTs[i % 2], hTs_b[i % 2], scr)

        sctr = [0]
        prep(0)
        for i, (S, ti, t0, T, only_ur) in enumerate(tiles):
            hT, hT_b = hTs[i % 2], hTs_b[i % 2]
            c0 = COL0[S] + t0
            panels = [2, 3] if only_ur else list(range(14))
            mid = len(panels) // 2
            for pi, pn in enumerate(panels):
                if pi == mid and i + 1 < len(tiles):
                    prep(i + 1)
                slot, slb = next_slot()
                load_w(w_in[l], 0, KC, pn * 512, 512, slot, slb)
                seg = pn // 2 if pn < 6 else (3 if pn < 10 else 4)
                sti = sctr[0] % 2
                for j in range(4):
                    n = pn * 4 + j
                    bank, bb = next_bank()
                    pairs = [(slot[:, k, j * 128:(j + 1) * 128], hT[:, k, 0:T]) for k in range(KC)]
                    mm_group(bank, bb, (0, T), pairs, [slb] + hT_b)
                    if seg == 0:
                        copy_op(ev_eng(), ufT[:, n, 0:T], bank[:, 0:T], [bb], [ufT_b[n]])
                    elif seg == 1:
                        copy_op("dve", stage[sti][:, j, 0:T], bank[:, 0:T], [bb], [stage_b[sti][j]])
                    elif seg == 2:
                        act_op(stage[sti][:, j, 0:T], bank[:, 0:T], AF.Gelu_apprx_tanh, [bb], [stage_b[sti][j]])
                    else:
                        act_op(stage[sti][:, j, 0:T], bank[:, 0:T], AF.Sigmoid, [bb], [stage_b[sti][j]])
                if seg == 0:
                    if pn == 1:
                        for tb in range(T // 128):
                            for g in range(4):
                                bank, bb = next_bank()
                                pairs = [(ufT[:, 2 * g + c2, tb * 128:(tb + 1) * 128], cs_sb[:, c2, :]) for c2 in range(2)]
                                mm_group(bank, bb, (0, 512), pairs, [ufT_b[2 * g], ufT_b[2 * g + 1], csb_b])
                                o = Pt[:, tb, :, g * 256:(g + 1) * 256]
                                iap = bank[:, 0:512].rearrange("p (c j) -> p c j", c=2)
                                eng = ev_eng()
                                if eng == "act":
                                    P.op("act", lambda e, o=o, i=iap: e.activation(out=o, in_=i, func=AF.Copy), reads=[bb], pwrites=[Pt_b])
                                else:
                                    P.op("dve", lambda e, o=o, i=iap: e.tensor_copy(out=o, in_=i), reads=[bb], pwrites=[Pt_b])
                        dma("sp", Pd[S][t0:t0 + T].rearrange("(a p) c j -> p a c j", p=128), Pt[:, 0:T // 128], Pd_b[S],
                            reads=[Pt_b], pwrites=[Pd_b[S]])
                else:
                    if seg == 1:
                        dst = urT[(pn - 2) * 4:(pn - 2) * 4 + 4]
                    elif seg == 2:
                        dst = ggT[(pn - 4) * 4:(pn - 4) * 4 + 4]
                    elif seg == 3:
                        dst = sgfT[(pn - 6) * 4:(pn - 6) * 4 + 4]
                    else:
                        dst = sgrT[(pn - 10) * 4:(pn - 10) * 4 + 4]
                    dma("sp", dst.rearrange("k p t -> p k t")[:, :, c0:c0 + T], stage[sti][:, :, 0:T], proj_b[S][ti],
                        reads=stage_b[sti], pwrites=[proj_b[S][ti]])
                    sctr[0] += 1

    def phaseB(l, hook=None):
        cv.reset()
        N = NTOK
        ur = cv.take(N, F32)
        gg = cv.take(N, F32)
        v = cv.take(N, F32)
        vbf = cv.take(N, BF16)
        r_ = cv.take(N, F32)
        i_ = cv.take(N, F32)
        a_ = cv.take(N, F32)
        m_ = cv.take(N, F32)
        hf = cv.take(N, F32)
        zst = cv.take(N, BF16)
        gwall = cv.take(8 * 4 * 128, BF16).rearrange("p (c g j) -> p c g j", c=8, g=4)
        v_b, vbf_b, r_b, i_b, a_b, m_b, hf_b, zst_b = [Buf(n) for n in ("v", "vbf", "r", "i", "a", "m", "hf", "zst")]
        ur_b, gg_b, gw_b = pb("ur"), pb("gg"), pb("gw")
        allproj = proj_b["ctx"] + proj_b["lat"]
        lb = l * P_LSZ
        blocks = [(0, CTX)] + [(CTX + t * TT, TT) for t in range(SEQ // TT)]
        for d_ in range(2):
            if d_ == 0:
                dma("pool", gwall[:, :, d_, :], lru_wa[l, d_].rearrange("c i j -> i c j"), gw_b, writes=[gw_b])
            else:
                dma("pool", gwall[:, :, d_, :], lru_wa[l, d_].rearrange("c i j -> i c j"), gw_b, pwrites=[gw_b])
            dma("pool", gwall[:, :, 2 + d_, :], lru_wx[l, d_].rearrange("c i j -> i c j"), gw_b, pwrites=[gw_b])
        for c in range(8):
            gw = gwall[:, c]
            dma("sp", ur[:], urT[c], ur_b, reads=allproj, writes=[ur_b])
            dma("sp", gg[:], ggT[c], gg_b, reads=allproj, writes=[gg_b])
            cwl = [par[:, lb + P_CW + k * 8 + c:lb + P_CW + k * 8 + c + 1] for k in range(4)]
            cw = lambda k, cwl=cwl: cwl[k]
            cb = par[:, lb + P_CB + c:lb + P_CB + c + 1]
            dve(lambda e, cw=cw, cb=cb: e.tensor_scalar(out=v[:], in0=ur[:], scalar1=cw(2), scalar2=cb, op0=ALU.mult, op1=ALU.add),
                [ur_b, par_b], [v_b])
            for k in (0, 1, 3):
                off = k - 2
                lo = max(0, -off)
                hi = max(0, off)
                dve(lambda e, cw=cw, k=k, lo=lo, hi=hi, off=off: e.scalar_tensor_tensor(
                    out=v[:, lo:CTX - hi], in0=ur[:, lo + off:CTX - hi + off], scalar=cw(k), in1=v[:, lo:CTX - hi],
                    op0=ALU.mult, op1=ALU.add), [ur_b, par_b, v_b], [v_b])
                v3 = v[:, CTX:N].rearrange("p (r w) -> p r w", w=64)
                u3 = ur[:, CTX:N].rearrange("p (r w) -> p r w", w=64)
                dve(lambda e, cw=cw, k=k, lo=lo, hi=hi, off=off, v3=v3, u3=u3: e.scalar_tensor_tensor(
                    out=v3[:, :, lo:64 - hi], in0=u3[:, :, lo + off:64 - hi + off], scalar=cw(k), in1=v3[:, :, lo:64 - hi],
                    op0=ALU.mult, op1=ALU.add), [ur_b, par_b, v_b], [v_b])
            copy_op("act", vbf[:], v[:], [v_b], [vbf_b])
            hb = None
            for d in range(2):
                for gi, (dst, dst_b, boff) in enumerate(((r_, r_b, P_BA), (i_, i_b, P_BX))):
                    bias = par[:, lb + boff + d * 8 + c:lb + boff + d * 8 + c + 1]
                    for bi, (b0, bw) in enumerate(blocks):
                        bank, bb = next_bank()
                        mm_group(bank, bb, (0, bw), [(gw[:, gi * 2 + d, :], vbf[:, b0:b0 + bw])], [gw_b, vbf_b])
                        P.op("act", lambda e, o=dst[:, b0:b0 + bw], i=bank[:, 0:bw], bias=bias: e.activation(out=o, in_=i, func=AF.Sigmoid, bias=bias),
                             reads=[bb, par_b], pwrites=[dst_b])
                sc1 = s8[:, l * 32 + d * 8 + c:l * 32 + d * 8 + c + 1]
                sc2 = s8[:, l * 32 + 16 + d * 8 + c:l * 32 + 16 + d * 8 + c + 1]
                act_op(a_[:], r_[:], AF.Exp, [r_b, s8_bufs[l]], [a_b], scale=sc1)
                act_op(m_[:], r_[:], AF.Exp, [r_b, s8_bufs[l]], [m_b], scale=sc2)
                act_op(m_[:], m_[:], AF.Relu, [m_b, misc_b], [m_b], bias=oneb[:], scale=-1.0)
                act_op(m_[:], m_[:], AF.Sqrt, [m_b], [m_b])
                dve(lambda e: e.tensor_tensor(out=i_[:], in0=i_[:], in1=v[:], op=ALU.mult), [i_b, v_b], [i_b])
                dve(lambda e: e.tensor_tensor(out=i_[:], in0=i_[:], in1=m_[:], op=ALU.mult), [i_b, m_b], [i_b])
                if d == 0:
                    dve(lambda e: e.tensor_tensor_scan(out=hf[:], data0=a_[:], data1=i_[:], initial=0.0, op0=ALU.mult, op1=ALU.add),
                        [a_b, i_b], [hf_b])
                else:
                    dve(lambda e: e.tensor_tensor_scan(out=r_[:, 0:CTX][:, ::-1], data0=a_[:, 0:CTX][:, ::-1], data1=i_[:, 0:CTX][:, ::-1],
                                                       initial=0.0, op0=ALU.mult, op1=ALU.add), [a_b, i_b, r_b], [r_b])
                    dve(lambda e: e.tensor_tensor_scan(out=r_[:, CTX:N][:, ::-1], data0=a_[:, CTX:N][:, ::-1], data1=i_[:, CTX:N][:, ::-1],
                                                       initial=r_[:, 0:1], op0=ALU.mult, op1=ALU.add), [a_b, i_b, r_b], [r_b])
            dve(lambda e: e.tensor_tensor(out=hf[:], in0=hf[:], in1=r_[:], op=ALU.add), [hf_b, r_b], [hf_b])
            dve(lambda e: e.tensor_tensor(out=zst[:], in0=hf[:], in1=gg[:], op=ALU.mult), [hf_b, gg_b], [zst_b])
            dma("sp", zTd[c], zst[:], zT_b, reads=[zst_b], pwrites=[zT_b])
            if hook is not None:
                hook()

    def phaseF(l, S):
        cv.reset()
        L = CTX if S == "ctx" else SEQ
        ntc = L // 128
        Psb = cv.take(ntc * 2048, BF16).rearrange("p (a c j) -> p a c j", a=ntc, c=2)
        Psb_b = pb("Psb")
        yst = [cv.take(8 * TT, BF16).rearrange("p (k t) -> p k t", k=8) for _ in range(2)]
        yst_b = [[Buf("yst%d_%d" % (i, k)) for k in range(8)] for i in range(2)]
        tp = tpc_d if S == "ctx" else tpl_d
        for a0 in range(0, ntc, 4):
            na = min(4, ntc - a0)
            dma("sp", Psb[:, a0:a0 + na], Pd[S][a0 * 128:(a0 + na) * 128].rearrange("(a p) c j -> p a c j", p=128), Psb_b,
                reads=[Pd_b[S]], pwrites=[Psb_b])
        for ti, (t0, T) in enumerate(tile_list(S)):
            slots = []
            for cs in range(2):
                slot, slb = next_slot()
                src = tp[cs, :, t0:t0 + T].rearrange("(a p) k -> p a k", p=128)
                dma("pool", slot[:, 0:ntc, 0:T], src, slb, writes=[slb], nobar=True)
                slots.append((slot, slb))
            si = ti % 2
            for jc in range(8):
                bank, bb = next_bank()
                pairs = []
                for cs in range(2):
                    for a in range(ntc):
                        pairs.append((Psb[:, a, cs, jc * 128:(jc + 1) * 128], slots[cs][0][:, a, 0:T]))
                mm_group(bank, bb, (0, T), pairs, [Psb_b, slots[0][1], slots[1][1]])
                copy_op(ev_eng(), yst[si][:, jc, 0:T], bank[:, 0:T], [bb], [yst_b[si][jc]])
            c0 = COL0[S] + t0
            dma("sp", YTd.rearrange("k p t -> p k t")[:, :, c0:c0 + T], yst[si][:, :, 0:T], YT_b[S][ti],
                reads=yst_b[si], writes=[YT_b[S][ti]])

    def phaseC(l, S, ti, t0, T):
        cv.reset()
        c0 = COL0[S] + t0
        yt = cv.take(8 * TT, BF16).rearrange("p (k t) -> p k t", k=8)
        zt = cv.take(8 * TT, BF16).rearrange("p (k t) -> p k t", k=8)
        sgf = [cv.take(4 * TT, F32).rearrange("p (j t) -> p j t", j=4) for _ in range(2)]
        sgr = [cv.take(4 * TT, F32).rearrange("p (j t) -> p j t", j=4) for _ in range(2)]
        t1 = [cv.take(TT, F32) for _ in range(2)]
        t2 = [cv.take(TT, F32) for _ in range(2)]
        mg = cv.take(KC * TT, BF16).rearrange("p (k t) -> p k t", k=KC)
        yt_b, zt_b = pb("yt"), pb("zt")
        sgf_b = [pb("sgf0"), pb("sgf1")]
        sgr_b = [pb("sgr0"), pb("sgr1")]
        t1_b = [Buf("t1_0"), Buf("t1_1")]
        t2_b = [Buf("t2_0"), Buf("t2_1")]
        mg_b = [Buf("mg%d" % k) for k in range(KC)]
        dma("sp", res[:, :, 0:T], resT[S].rearrange("k p t -> p k t")[:, :, t0:t0 + T], res_b[0],
            reads=[resT_b[S][ti]], writes=res_b, nobar=True)
        dma("sp", yt[:, :, 0:T], YTd.rearrange("k p t -> p k t")[:, :, c0:c0 + T], yt_b, reads=[YT_b[S][ti]], writes=[yt_b])
        dma("sp", zt[:, :, 0:T], zTd.rearrange("k p t -> p k t")[:, :, c0:c0 + T], zt_b, reads=[zT_b], writes=[zt_b])
        for pn in range(4):
            slot, slb = next_slot()
            load_w(w_fo[l], 0, 8, pn * 512, 512, slot, slb, kc_off=0, first=True)
            load_w(w_ro[l], 0, 8, pn * 512, 512, slot, slb, kc_off=8, first=False)
            si = pn % 2
            dma("sp", sgf[si][:, :, 0:T], sgfT[pn * 4:pn * 4 + 4].rearrange("k p t -> p k t")[:, :, c0:c0 + T], sgf_b[si],
                reads=[proj_b[S][ti]], writes=[sgf_b[si]])
            dma("sp", sgr[si][:, :, 0:T], sgrT[pn * 4:pn * 4 + 4].rearrange("k p t -> p k t")[:, :, c0:c0 + T], sgr_b[si],
                reads=[proj_b[S][ti]], writes=[sgr_b[si]])
            for j in range(4):
                n = pn * 4 + j
                i2 = n % 2
                bF, bFb = next_bank()
                bR, bRb = next_bank()
                mm_group(bF, bFb, (0, T), [(slot[:, k, j * 128:(j + 1) * 128], yt[:, k, 0:T]) for k in range(8)], [slb, yt_b])
                mm_group(bR, bRb, (0, T), [(slot[:, 8 + k, j * 128:(j + 1) * 128], zt[:, k, 0:T]) for k in range(8)], [slb, zt_b])
                dve(lambda e, o=t1[i2], b=bF, s=sgf[si], j=j: e.tensor_tensor(out=o[:, 0:T], in0=b[:, 0:T], in1=s[:, j, 0:T], op=ALU.mult),
                    [bFb, sgf_b[si]], [t1_b[i2]])
                dve(lambda e, o=t2[i2], b=bR, s=sgr[si], j=j: e.tensor_tensor(out=o[:, 0:T], in0=b[:, 0:T], in1=s[:, j, 0:T], op=ALU.mult),
                    [bRb, sgr_b[si]], [t2_b[i2]])
                dve(lambda e, n=n, i2=i2: e.tensor_tensor(out=mg[:, n, 0:T], in0=t1[i2][:, 0:T], in1=t2[i2][:, 0:T], op=ALU.add),
                    [t1_b[i2], t2_b[i2]], [mg_b[n]])
        g1 = modv(l, S, 2)
        for pn in range(4):
            slot, slb = next_slot()
            load_w(w_o[l], 0, KC, pn * 512, 512, slot, slb)
            for j in range(4):
                dch = pn * 4 + j
                bank, bb = next_bank()
                mm_group(bank, bb, (0, T), [(slot[:, k, j * 128:(j + 1) * 128], mg[:, k, 0:T]) for k in range(KC)], [slb] + mg_b)
                dve(lambda e, dch=dch, bank=bank: e.scalar_tensor_tensor(out=res[:, dch, 0:T], in0=bank[:, 0:T], scalar=g1[:, dch:dch + 1],
                                                                          in1=res[:, dch, 0:T], op0=ALU.mult, op1=ALU.add),
                    [bb, mod_b, res_b[dch]], [res_b[dch]])

    def swiglu_expert(T, h2, h2_b, Wg, Wu, Wd, nf, actT, actT_b, epilogue):
        sg = [cv_sg[0], cv_sg[1]]
        npan = nf // 4
        for fp in range(npan):
            sG, sGb = next_slot()
            load_w(Wg, 0, KC, fp * 512, 512, sG, sGb)
            sU, sUb = next_slot()
            load_w(Wu, 0, KC, fp * 512, 512, sU, sUb)
            for j in range(4):
                f = fp * 4 + j
                i2 = f % 2
                bG, bGb = next_bank()
                bU, bUb = next_bank()
                mm_group(bG, bGb, (0, T), [(sG[:, k, j * 128:(j + 1) * 128], h2[:, k, 0:T]) for k in range(KC)], [sGb] + h2_b)
                mm_group(bU, bUb, (0, T), [(sU[:, k, j * 128:(j + 1) * 128], h2[:, k, 0:T]) for k in range(KC)], [sUb] + h2_b)
                act_op(sg[i2][:, 0:T], bG[:, 0:T], AF.Silu, [bGb], [cv_sg_b[i2]])
                dve(lambda e, f=f, i2=i2, bU=bU: e.tensor_tensor(out=actT[:, f, 0:T], in0=bU[:, 0:T], in1=sg[i2][:, 0:T], op=ALU.mult),
                    [bUb, cv_sg_b[i2]], [actT_b[f]])
        slabs = []
        k0 = 0
        while k0 < nf:
            nk = min(14 if nf % 14 == 0 else 16, nf - k0)
            slabs.append((k0, nk))
            k0 += nk
        for dp in range(4):
            banks = [next_bank() for _ in range(4)]
            for si, (k0, nk) in enumerate(slabs):
                slot, slb = next_slot()
                load_w(Wd, k0, nk, dp * 512, 512, slot, slb)
                for j in range(4):
                    bank, bb = banks[j]
                    mm_group(bank, bb, (0, T), [(slot[:, k, j * 128:(j + 1) * 128], actT[:, k0 + k, 0:T]) for k in range(nk)],
                             [slb] + actT_b[k0:k0 + nk], first=(si == 0), last=(si == len(slabs) - 1))
            for j in range(4):
                epilogue(dp * 4 + j, banks[j][0], banks[j][1])

    cv_sg = [None, None]
    cv_sg_b = [Buf("sg0"), Buf("sg1")]

    def phaseD(l, S, ti, t0, T):
        cv.reset()
        last = (l == DEPTH - 1)
        moe = (l % 2 == 1)
        h2 = cv.take(KC * TT, BF16).rearrange("p (k t) -> p k t", k=KC)
        h2_b = [Buf("h2_%d" % k) for k in range(KC)]
        scr = norm_scratch()
        cv_sg[0] = cv.take(TT, F32)
        cv_sg[1] = cv.take(TT, F32)
        nf = (D_EXP if moe else D_FF) // 128
        actT = cv.take(nf * TT, BF16).rearrange("p (k t) -> p k t", k=nf)
        actT_b = [Buf("act%d" % k) for k in range(nf)]
        g2 = modv(l, S, 5)
        if not moe:
            norm_mod(T, drvA(l, S, 1), modv(l, S, 3), h2, h2_b, scr)

            def epi(dch, bank, bb):
                dve(lambda e, dch=dch, bank=bank: e.scalar_tensor_tensor(out=res[:, dch, 0:T], in0=bank[:, 0:T], scalar=g2[:, dch:dch + 1],
                                                                          in1=res[:, dch, 0:T], op0=ALU.mult, op1=ALU.add),
                    [bb, mod_b, res_b[dch]], [res_b[dch]])
            swiglu_expert(T, h2, h2_b, ffn_g[0], ffn_u[0], ffn_d[0], nf, actT, actT_b, epi)
        else:
            ntb = T // 128
            Bsb = cv.take(2 * TT, F32).rearrange("p (e t) -> p e t", e=2)
            Bsb_b = [Buf("Bsb0"), Buf("Bsb1")]
            h2f = scr[0]
            h2f_b = scr[1]
            diag = [cv.take(128, F32) for _ in range(2)]
            diag_b = [Buf("diag0"), Buf("diag1")]
            sm = cv.take(64, F32)
            sm_b = Buf("sm")
            comb = cv.take(ntb * NEXP, F32).rearrange("p (a e) -> p a e", a=ntb)
            comb_b = [Buf("comb%d" % a) for a in range(ntb)]
            tmp = scr[2]
            tmp_b = scr[3]
            lbanks = [next_bank() for _ in range(ntb)]
            Bv = modv(l, S, 3)

            def extra(k, xs_ap, xs_buf):
                i2 = k % 2
                act_op(h2f[i2][:, 0:T], xs_ap[:, 0:T], AF.Identity, [xs_buf, mod_b], [h2f_b[i2]], bias=Bv[:, k:k + 1])
                copy_op("dve", h2[:, k, 0:T], h2f[i2][:, 0:T], [h2f_b[i2]], [h2_b[k]])
                for a in range(ntb):
                    bank, bb = lbanks[a]
                    mm_group(bank, bb, (0, NEXP), [(h2f[i2][:, a * 128:(a + 1) * 128], router_sb[:, k, :])], [h2f_b[i2], rt_b],
                             first=(k == 0), last=(k == KC - 1))
            norm_mod(T, drvA(l, S, 1), None, h2, h2_b, scr, extra=extra)
            for a in range(ntb):
                bank, bb = lbanks[a]
                lg = sm[:, 0:8]
                eq1 = sm[:, 8:16]
                lg2 = sm[:, 16:24]
                eq2 = sm[:, 24:32]
                m1 = sm[:, 32:33]
                m2 = sm[:, 33:34]
                ee = sm[:, 34:35]
                g1_ = sm[:, 35:36]
                g2_ = sm[:, 36:37]
                c1 = sm[:, 40:48]
                R = [sm_b]
                dve(lambda e, bank=bank: e.tensor_copy(out=lg, in_=bank[:, 0:8]), [bb, sm_b], R)
                dve(lambda e: e.reduce_max(out=m1, in_=lg, axis=AX.X), R, R)
                dve(lambda e: e.tensor_scalar(out=eq1, in0=lg, scalar1=m1, scalar2=None, op0=ALU.is_equal), R, R)
                dve(lambda e: e.scalar_tensor_tensor(out=lg2, in0=eq1, scalar=-1e30, in1=lg, op0=ALU.mult, op1=ALU.add), R, R)
                dve(lambda e: e.reduce_max(out=m2, in_=lg2, axis=AX.X), R, R)
                dve(lambda e: e.tensor_scalar(out=eq2, in0=lg2, scalar1=m2, scalar2=None, op0=ALU.is_equal), R, R)
                dve(lambda e: e.tensor_tensor(out=ee, in0=m2, in1=m1, op=ALU.subtract), R, R)
                act_op(ee, ee, AF.Exp, R, R)
                dve(lambda e: e.tensor_scalar(out=g1_, in0=ee, scalar1=1.0, scalar2=None, op0=ALU.add), R, R)
                dve(lambda e: e.reciprocal(out=g1_, in_=g1_), R, R)
                dve(lambda e: e.tensor_tensor(out=g2_, in0=ee, in1=g1_, op=ALU.mult), R, R)
                dve(lambda e: e.tensor_scalar(out=c1, in0=eq1, scalar1=g1_, scalar2=None, op0=ALU.mult), R, R)
                dve(lambda e, a=a: e.scalar_tensor_tensor(out=comb[:, a, :], in0=eq2, scalar=g2_, in1=c1, op0=ALU.mult, op1=ALU.add),
                    R, [comb_b[a]])
            dctr = 0
            ectr2 = [0]
            for ex in range(NEXP):
                bank, bb = next_bank()
                bx = ex % 2
                for a in range(ntb):
                    i2 = dctr % 2
                    dctr += 1
                    dve(lambda e, i2=i2, a=a, ex=ex: e.tensor_scalar(out=diag[i2][:], in0=ident[:], scalar1=comb[:, a, ex:ex + 1], scalar2=None, op0=ALU.mult),
                        [comb_b[a], consts_b], [diag_b[i2]])
                    mm_group(bank, bb, (a * 128, (a + 1) * 128), [(ones[:], diag[i2][:])], [diag_b[i2], misc_b], fresh=(a == 0))
                copy_op("act", Bsb[:, bx, 0:T], bank[:, 0:T], [bb], [Bsb_b[bx]])

                def epi(dch, bank, bb, bx=bx):
                    i2 = ectr2[0] % 2
                    ectr2[0] += 1
                    dve(lambda e, dch=dch, bank=bank, i2=i2, bx=bx: e.scalar_tensor_tensor(out=tmp[i2][:, 0:T], in0=bank[:, 0:T], scalar=g2[:, dch:dch + 1],
                                                                                            in1=Bsb[:, bx, 0:T], op0=ALU.mult, op1=ALU.mult),
                        [bb, mod_b, Bsb_b[bx]], [tmp_b[i2]])
                    dve(lambda e, dch=dch, i2=i2: e.tensor_tensor(out=res[:, dch, 0:T], in0=res[:, dch, 0:T], in1=tmp[i2][:, 0:T], op=ALU.add),
                        [tmp_b[i2], res_b[dch]], [res_b[dch]])
                swiglu_expert(T, h2, h2_b, moe_g[0, ex], moe_u[0, ex], moe_d[0, ex], nf, actT, actT_b, epi)
        if not last:
            dma("sp", resT[S].rearrange("k p t -> p k t")[:, :, t0:t0 + T], res[:, :, 0:T], resT_b[S][ti],
                reads=res_b, writes=[resT_b[S][ti]])
        elif S == "lat":
            P.barrier()
            cv.reset()
            scr = norm_scratch()
            sq, sq_b, xs, xs_b, rstd, rstd_b = scr
            yf = cv.take(KC * TT, F32).rearrange("p (k t) -> p k t", k=KC)
            yf_b = [Buf("yf%d" % k) for k in range(KC)]
            otok = [cv.take(D, F32) for _ in range(2)]
            otok_b = [[Buf("otok%d_%d" % (i, q)) for q in range(4)] for i in range(2)]
            bank, bb = next_bank()
            for k in range(KC):
                i2 = k % 2
                act_op(sq[i2][:, 0:T], res[:, k, 0:T], AF.Square, [res_b[k]], [sq_b[i2]])
                mm_group(bank, bb, (0, T), [(ones[:], sq[i2][:, 0:T])], [sq_b[i2], misc_b], first=(k == 0), last=(k == KC - 1))
            act_op(rstd[:, 0:T], bank[:, 0:T], AF.Sqrt, [bb, misc_b], [rstd_b], bias=epsb[:], scale=1.0 / D)
            dve(lambda e: e.reciprocal(out=rstd[:, 0:T], in_=rstd[:, 0:T]), [rstd_b], [rstd_b])
            gfin = par[:, P_FIN:P_FIN + 16]
            for k in range(KC):
                dve(lambda e, k=k: e.scalar_tensor_tensor(out=yf[:, k, 0:T], in0=res[:, k, 0:T], scalar=gfin[:, k:k + 1], in1=rstd[:, 0:T],
                                                          op0=ALU.mult, op1=ALU.mult), [res_b[k], rstd_b, par_b], [yf_b[k]])
            for a in range(T // 128):
                oi = a % 2
                for q in range(4):
                    bank, bb = next_bank()

                    def fn(e, bank=bank, a=a, q=q):
                        ins = None
                        for kk in range(4):
                            k = q * 4 + kk
                            ins = e.transpose(bank[:, kk * 128:(kk + 1) * 128], yf[:, k, a * 128:(a + 1) * 128], ident[:])
                        return ins
                    P.op("pe", fn, reads=yf_b[q * 4:q * 4 + 4] + [consts_b], writes=[bb])
                    copy_op(ev_eng(), otok[oi][:, q * 512:(q + 1) * 512], bank[:, 0:512], [bb], [otok_b[oi][q]])
                dma("sp", out_d[t0 + a * 128:t0 + (a + 1) * 128, :], otok[oi][:], out_b, reads=otok_b[oi], pwrites=[out_b])

    stages = []

    def stage(name):
        stages.append(name)
        return stop_after is not None and len(stages) > stop_after

    def run():
        ada0 = ada_steps(0)
        phase0(hook=lambda: run_steps(ada0, 6))
        run_steps(ada0, 100)
        P.barrier()
        if stage("p0ada"):
            return
        for l in range(DEPTH):
            last = (l == DEPTH - 1)
            phaseA_all(l)
            P.barrier()
            if stage("A%d" % l):
                return
            if not last:
                adan = ada_steps(l + 1)
                phaseB(l, hook=lambda: run_steps(adan, 4))
                run_steps(adan, 100)
            else:
                phaseB(l)
            P.barrier()
            if dump and last:
                dma("sp", mod_dbg, mod[:], pb("moddbg"), reads=[mod_b])
            if stage("B%d" % l):
                return
            for S in ("ctx", "lat"):
                if last and S == "ctx":
                    continue
                phaseF(l, S)
                P.barrier()
            if stage("F%d" % l):
                return
            for S in ("ctx", "lat"):
                if last and S == "ctx":
                    continue
                for ti, (t0, T) in enumerate(tile_list(S)):
                    phaseC(l, S, ti, t0, T)
                    P.barrier()
                    phaseD(l, S, ti, t0, T)
                    P.barrier()
            if stage("CD%d" % l):
                return

    run()
    fin_reads = [out_b, zT_b] + [b for S in ("ctx", "lat") for b in resT_b[S] + proj_b[S] + YT_b[S]] + [Pd_b["ctx"], Pd_b["lat"]]
    P.op("sp", None, reads=fin_reads, barrier=True)

    all_bufs_with_dma = set()
    for e in P.ENGS:
        for o in P.q[e]:
            if o.dma:
                all_bufs_with_dma.add(o.dst)
    for i, b in enumerate(sorted(all_bufs_with_dma, key=lambda b: b.name)):
        b.dsem = es.enter_context(nc.semaphore("d%d_%s" % (i, b.name)))
    sems = {e: es.enter_context(nc.semaphore("eng_" + e)) for e in P.ENGS}
    with nc.Block() as block:
        @block.tensor
        def _(e):
            P.emit_one("pe", e, sems)

        @block.scalar
        def _(e):
            P.emit_one("act", e, sems)

        @block.vector
        def _(e):
            P.emit_one("dve", e, sems)

        @block.gpsimd
        def _(e):
            P.emit_one("pool", e, sems)

        @block.sync
        def _(e):
            P.emit_one("sp", e, sems)
    es.close()
    return nc, stages


def _prog_mark(self):
    for e in self.ENGS:
        for o in self.q[e]:
            for d in o.raw + o.war:
                if not d.dma:
                    d.sig = True
    for e in self.ENGS:
        n = 0
        for o in self.q[e]:
            if o.sig and not o.dma:
                n += 1
                o.idx = n
    self.marked = True


def _prog_emit_one(self, e, eng, sems):
    if not getattr(self, "marked", False):
        _prog_mark(self)
    waited = {}

    def need(sem, val):
        if waited.get(sem.name, 0) < val:
            eng.wait_ge(sem, val)
            waited[sem.name] = val

    for o in self.q[e]:
        for lst, is_war in ((o.raw, False), (o.war, True)):
            for d in lst:
                if d.dma:
                    need(d.dst.dsem, d.dval)
                else:
                    if d.eng == e and (e in ("pe", "sp", "pool") or is_war):
                        continue
                    need(sems[d.eng], d.idx)
        ins = o.fn(eng) if o.fn is not None else None
        if o.dma:
            ins.then_inc(o.dst.dsem, 16)
        elif o.sig:
            if ins is None:
                ins = eng.nop()
            ins.then_inc(sems[e], 1)


Prog.emit_one = _prog_emit_one


def _host_consts():
    j = np.arange(256)
    ang = 2.0 * np.pi * ((j[:, None] * j[None, :]) % 256) / 256.0
    cs256 = np.concatenate([np.cos(ang), np.sin(ang)], axis=1) / 16.0

    def tp(L):
        t = np.arange(L)
        a = 2.0 * np.pi * ((t[:, None] * t[None, :]) % L) / float(L)
        return np.stack([np.cos(a), -np.sin(a)], axis=0) / np.sqrt(float(L))
    return (np.ascontiguousarray(cs256, dtype=np.float32), np.ascontiguousarray(tp(SEQ), dtype=np.float32),
            np.ascontiguousarray(tp(CTX), dtype=np.float32), np.eye(128, dtype=np.float32))


def _fm(vec, nchunk):
    return np.ascontiguousarray(np.asarray(vec, dtype=np.float32).reshape(nchunk, 128).T)


def _params(inp):
    par = np.zeros((128, NPAR), np.float32)
    for l in range(DEPTH):
        b = l * P_LSZ
        par[:, b + P_ADAB:b + P_ADAB + 96] = _fm(inp["ada_b"][l], 96)
        par[:, b + P_N1:b + P_N1 + 16] = _fm(inp["norm1_g"][l], 16)
        par[:, b + P_N2:b + P_N2 + 16] = _fm(inp["norm2_g"][l], 16)
        for k in range(4):
            par[:, b + P_CW + k * 8:b + P_CW + k * 8 + 8] = _fm(inp["conv_w"][l, k], 8)
        par[:, b + P_CB:b + P_CB + 8] = _fm(inp["conv_b"][l], 8)
        for d in range(2):
            par[:, b + P_BA + d * 8:b + P_BA + d * 8 + 8] = _fm(inp["lru_ba"][l, d], 8)
            par[:, b + P_BX + d * 8:b + P_BX + d * 8 + 8] = _fm(inp["lru_bx"][l, d], 8)
            par[:, b + P_LAM + d * 8:b + P_LAM + d * 8 + 8] = _fm(inp["lru_lambda"][l, d], 8)
    par[:, P_FIN:P_FIN + 16] = _fm(inp["final_norm_g"], 16)
    return par


_CACHE = {}


def make_in_maps(inp):
    cs256, tpl, tpc, ident = _host_consts()
    par = _params(inp)
    shared = {"params": par, "ident": ident, "cs256": cs256, "tp_lat": tpl, "tp_ctx": tpc}
    for name in ("ada_w", "w_in", "lru_wa", "lru_wx", "w_fourier_out", "w_lru_out", "w_out", "ffn_w_gate", "ffn_w_up",
                 "ffn_w_down", "moe_router", "moe_w_gate", "moe_w_up", "moe_w_down"):
        shared[name] = np.ascontiguousarray(np.asarray(inp[name], dtype=np.float32))
    maps = []
    for b in range(8):
        m = dict(shared)
        m["x"] = np.ascontiguousarray(np.asarray(inp["x"][b], dtype=np.float32))
        m["ctx"] = np.ascontiguousarray(np.asarray(inp["ctx"][b], dtype=np.float32))
        cc = np.stack([_fm(inp["c"][b], KC), _fm(inp["c_ctx"], KC)], axis=-1)
        m["cc"] = np.ascontiguousarray(cc, dtype=np.float32)
        maps.append(m)
    return maps


def kernel(**inputs):
    if "nc" not in _CACHE:
        _CACHE["nc"] = build_program()[0]
    nc = _CACHE["nc"]
    in_maps = make_in_maps(inputs)
    r = run_bass_kernel_spmd(nc, in_maps, core_ids=list(range(8)))
    return np.stack([np.asarray(r.results[b]["out"], dtype=np.float32) for b in range(8)], axis=0)
```
